# Optimizing a Trainium2 kernel written in Bass

```python
import jax
import jax.numpy as jnp
from jax import lax
import numpy as np

D_MODEL = 1024
BATCH = 16
SEQ = 4096
DEPTH = 2

F32 = jnp.float32
GRID_W = 64
CTX_LEN = 256
EPS = 1e-6

LRU_WIDTH = D_MODEL // 2
LRU_BLOCKS = 8
LRU_BLOCK = LRU_WIDTH // LRU_BLOCKS
LRU_C = 8.0
CONV_W = 4
CONV_PAD = (2, 1)
GLA_HEADS = 4
GLA_DK = 64
GLA_DV = 128
GLA_KEY = GLA_HEADS * GLA_DK
GLA_VAL = GLA_HEADS * GLA_DV
GLA_RANK = 16
GLA_TAU = 16.0
GLA_CHUNK = 64
MIX_WIDTH = LRU_WIDTH + GLA_VAL
EVEN_SPLITS = (LRU_WIDTH, LRU_WIDTH, GLA_KEY, GLA_KEY, GLA_VAL, GLA_VAL, GLA_RANK, GLA_RANK)
EVEN_IN = sum(EVEN_SPLITS)
ATT_HEADS = 8
KV_HEADS = 2
Q_PER_KV = ATT_HEADS // KV_HEADS
HEAD_DIM = 128
ROPE_THETA = 10000.0
Q_BLOCK = 128
QKV_SPLITS = (ATT_HEADS * HEAD_DIM, KV_HEADS * HEAD_DIM, KV_HEADS * HEAD_DIM)
QKV_WIDTH = sum(QKV_SPLITS)
N_GROUPS = 4
EXPERTS_PER_GROUP = 4
N_EXPERTS = N_GROUPS * EXPERTS_PER_GROUP
EXPERT_HIDDEN = 512
TOP_K = 2

kernel_name = "hybrid_rglru_gla_gqa_hmoe_dit"


def _split(z, sizes):
    cuts = np.cumsum(sizes)[:-1].tolist()
    return jnp.split(z, cuts, axis=-1)


def rmsnorm(x, g):
    xf = x.astype(F32)
    y = xf * lax.rsqrt(jnp.mean(xf * xf, axis=-1, keepdims=True) + EPS)
    return (y * g.astype(F32)).astype(x.dtype)


def modulate(h, shift, scale):
    return h * (1 + scale) + shift


def adaln(cvec, w, b):
    m = jax.nn.silu(cvec) @ w + b
    m = m.reshape(m.shape[0], 6, 1, D_MODEL)
    return tuple(m[:, j] for j in range(6))


def dwconv(u, w, b):
    y = lax.conv_general_dilated(u, w[:, None, :], window_strides=(1,), padding=[CONV_PAD],
                                 dimension_numbers=("NWC", "WIO", "NWC"),
                                 feature_group_count=u.shape[-1])
    return y + b


def linear_scan(a, b, h0):
    b = b.at[:, 0].add(a[:, 0] * h0)

    def combine(left, right):
        al, bl = left
        ar, br = right
        return al * ar, ar * bl + br

    _, h = lax.associative_scan(combine, (a, b), axis=1)
    return h


def rglru_coeffs(u, wa, ba, wi, bi, lam):
    ub = u.reshape(*u.shape[:-1], LRU_BLOCKS, LRU_BLOCK)
    r = jax.nn.sigmoid(jnp.einsum("blnc,ncd->blnd", ub, wa.astype(F32)).reshape(u.shape) + ba.astype(F32))
    ig = jax.nn.sigmoid(jnp.einsum("blnc,ncd->blnd", ub, wi.astype(F32)).reshape(u.shape) + bi.astype(F32))
    log_a = -LRU_C * r * jax.nn.softplus(-lam.astype(F32))
    a = jnp.exp(log_a)
    b = jnp.sqrt(-jnp.expm1(2.0 * log_a)) * (ig * u)
    return a, b


def gla_chunked(q, k, v, lg, s0):
    b_, n_tok = q.shape[:2]
    n_chunks = n_tok // GLA_CHUNK

    def chunk(t):
        return t.reshape(b_, n_chunks, GLA_CHUNK, *t.shape[2:])

    q, k, v, lg = chunk(q), chunk(k), chunk(v), chunk(lg)
    g = jnp.cumsum(lg, axis=2)
    g_last = g[:, :, -1:]
    qg = q * jnp.exp(g)
    kg = k * jnp.exp(-g)
    kd = k * jnp.exp(g_last - g)
    mask = jnp.tril(jnp.ones((GLA_CHUNK, GLA_CHUNK), bool))
    att = jnp.where(mask, jnp.einsum("bnihd,bnjhd->bnhij", qg, kg), 0.0)
    o_intra = jnp.einsum("bnhij,bnjhe->bnihe", att, v)
    ds = jnp.einsum("bnjhd,bnjhe->bnhde", kd, v)
    decay = jnp.exp(g_last[:, :, 0])

    def step(s, inp):
        dec, d_s = inp
        return dec[..., None] * s + d_s, s

    s_fin, s_prev = lax.scan(step, s0, (jnp.moveaxis(decay, 1, 0), jnp.moveaxis(ds, 1, 0)))
    s_prev = jnp.moveaxis(s_prev, 0, 1)
    o_inter = jnp.einsum("bnihd,bnhde->bnihe", qg, s_prev)
    return (o_intra + o_inter).reshape(b_, n_tok, GLA_HEADS, GLA_DV), s_fin


def rglru_gla_mixer(hc, hl, w_in, conv_w, conv_b, lru_wa, lru_ba, lru_wi, lru_bi, lru_lam,
                    gla_wg, gla_bg, gla_norm, w_out, ctx_out):
    dt = hl.dtype
    b_ = hl.shape[0]

    def project(h):
        n = h.shape[1]
        xa, ga, q, k, v, r, lr_f, lr_b = _split(h @ w_in, EVEN_SPLITS)
        u = dwconv(xa, conv_w, conv_b).astype(F32)
        q = q.astype(F32).reshape(b_, n, GLA_HEADS, GLA_DK) * GLA_DK ** -0.5
        k = k.astype(F32).reshape(b_, n, GLA_HEADS, GLA_DK)
        v = v.astype(F32).reshape(b_, n, GLA_HEADS, GLA_DV)
        return u, ga, q, k, v, r, (lr_f, lr_b)

    def direction(p, d):
        u, _, q, k, v, _, lrs = p
        a, b = rglru_coeffs(u, lru_wa[d], lru_ba[d], lru_wi[d], lru_bi[d], lru_lam[d])
        lg = jax.nn.log_sigmoid((lrs[d] @ gla_wg[d] + gla_bg[d]).astype(F32)) / GLA_TAU
        seqs = (a, b, q, k, v, lg.reshape(k.shape))
        if d == 1:
            seqs = tuple(jnp.flip(t, axis=1) for t in seqs)
        return seqs

    pc, pl = project(hc), project(hl)
    lru_c, lru_l, gla_c, gla_l = [], [], [], []
    for d in range(2):
        ac, bc, qc, kc, vc, gc = direction(pc, d)
        al, bl, ql, kl, vl, gl = direction(pl, d)
        h_c = linear_scan(ac, bc, jnp.zeros((b_, LRU_WIDTH), F32))
        h_l = linear_scan(al, bl, h_c[:, -1])
        o_c, s_c = gla_chunked(qc, kc, vc, gc, jnp.zeros((b_, GLA_HEADS, GLA_DK, GLA_DV), F32))
        o_l, _ = gla_chunked(ql, kl, vl, gl, s_c)
        if d == 1:
            h_c, h_l, o_c, o_l = (jnp.flip(t, axis=1) for t in (h_c, h_l, o_c, o_l))
        lru_c.append(h_c)
        lru_l.append(h_l)
        gla_c.append(o_c)
        gla_l.append(o_l)

    def finish(p, lru, gla):
        _, ga, _, _, _, r, _ = p
        n = lru.shape[1]
        ya = lru * jax.nn.gelu(ga.astype(F32))
        yb = rmsnorm(gla, gla_norm).reshape(b_, n, GLA_VAL) * jax.nn.silu(r.astype(F32))
        return jnp.concatenate([ya, yb], axis=-1).astype(dt) @ w_out

    yl = finish(pl, lru_l[0] + lru_l[1], gla_l[0] + gla_l[1])
    yc = finish(pc, lru_c[0] + lru_c[1], gla_c[0] + gla_c[1]) if ctx_out else None
    return yc, yl


def rope_tables(rows):
    row = jnp.repeat(jnp.arange(rows, dtype=F32), GRID_W)
    col = jnp.tile(jnp.arange(GRID_W, dtype=F32), rows)
    pairs_per_axis = HEAD_DIM // 4
    freqs = ROPE_THETA ** (-jnp.arange(pairs_per_axis, dtype=F32) / pairs_per_axis)
    ang = jnp.concatenate([row[:, None] * freqs, col[:, None] * freqs], axis=-1)
    return jnp.cos(ang), jnp.sin(ang)


def apply_rope(x, cos, sin):
    xf = x.astype(F32).reshape(*x.shape[:-1], HEAD_DIM // 2, 2)
    x1, x2 = xf[..., 0], xf[..., 1]
    out = jnp.stack([x1 * cos - x2 * sin, x1 * sin + x2 * cos], axis=-1)
    return out.reshape(x.shape).astype(x.dtype)


def attend(q, k, v):
    s = jnp.einsum("bqhgd,bkhd->bhgqk", q, k, preferred_element_type=F32) * HEAD_DIM ** -0.5
    p = jax.nn.softmax(s, axis=-1)
    return jnp.einsum("bhgqk,bkhd->bqhgd", p, v.astype(F32)).astype(v.dtype)


def gqa_mixer(hc, hl, w_qkv, q_norm, k_norm, w_o, cos, sin, ctx_out):
    b_ = hl.shape[0]

    def project(h):
        n = h.shape[1]
        q, k, v = _split(h @ w_qkv, QKV_SPLITS)
        q = rmsnorm(q.reshape(b_, n, KV_HEADS, Q_PER_KV, HEAD_DIM), q_norm)
        k = rmsnorm(k.reshape(b_, n, KV_HEADS, HEAD_DIM), k_norm)
        return q, k, v.reshape(b_, n, KV_HEADS, HEAD_DIM)

    qc, kc, vc = project(hc)
    ql, kl, vl = project(hl)
    ql = apply_rope(ql, cos[:, None, None], sin[:, None, None])
    kl = apply_rope(kl, cos[:, None], sin[:, None])
    k_all = jnp.concatenate([kc, kl], axis=1)
    v_all = jnp.concatenate([vc, vl], axis=1)
    n = hl.shape[1]
    qb = jnp.moveaxis(ql.reshape(b_, n // Q_BLOCK, Q_BLOCK, KV_HEADS, Q_PER_KV, HEAD_DIM), 1, 0)
    ob = lax.map(lambda qi: attend(qi, k_all, v_all), qb)
    yl = jnp.moveaxis(ob, 0, 1).reshape(b_, n, ATT_HEADS * HEAD_DIM) @ w_o
    yc = attend(qc, kc, vc).reshape(b_, hc.shape[1], ATT_HEADS * HEAD_DIM) @ w_o if ctx_out else None
    return yc, yl


def hier_moe(h, wg, bg, we, be, w1, w3, w2):
    b_, n, d = h.shape
    t = h.reshape(-1, d)
    n_tok = t.shape[0]
    p_group = jax.nn.softmax((t @ wg + bg).astype(F32), axis=-1)
    w_group, g_idx = lax.top_k(p_group, 1)
    e_logits = (t @ we + be).astype(F32).reshape(n_tok, N_GROUPS, EXPERTS_PER_GROUP)
    e_sel = e_logits[jnp.arange(n_tok), g_idx[:, 0]]
    e_val, e_idx = lax.top_k(e_sel, TOP_K)
    w_exp = jax.nn.softmax(e_val, axis=-1) * w_group
    global_idx = g_idx * EXPERTS_PER_GROUP + e_idx
    comb = jnp.sum(jax.nn.one_hot(global_idx, N_EXPERTS, dtype=F32) * w_exp[..., None], axis=1)
    out = jnp.zeros((n_tok, d), F32)
    for e in range(N_EXPERTS):
        he = jax.nn.silu(t @ w1[e]) * (t @ w3[e])
        out = out + comb[:, e:e + 1] * (he @ w2[e]).astype(F32)
    return out.astype(h.dtype).reshape(b_, n, d)


def setup_inputs(seed: int = 0) -> dict:
    key = jax.random.key(seed)
    ks = iter(jax.random.split(key, 48))
    d = D_MODEL
    n_even = (DEPTH + 1) // 2
    n_odd = DEPTH // 2

    def nrm(shape, fan_in, scale=1.0):
        return jax.random.normal(next(ks), shape, F32) * (scale * fan_in ** -0.5)

    def gain(shape):
        return 1.0 + 0.05 * jax.random.normal(next(ks), shape, F32)

    def bias(shape, s=0.02):
        return s * jax.random.normal(next(ks), shape, F32)

    x = jax.random.normal(next(ks), (BATCH, SEQ, d), F32)
    c = jax.random.normal(next(ks), (BATCH, d), F32)
    ctx = jax.random.normal(next(ks), (BATCH, CTX_LEN, d), F32)
    c_ctx = jax.random.normal(next(ks), (d,), F32)
    norm1 = gain((DEPTH, d))
    norm2 = gain((DEPTH, d))
    w_ada = nrm((DEPTH, d, 6 * d), d, 0.5)
    b_ada = bias((DEPTH, 6 * d))
    ev_w_in = nrm((n_even, d, EVEN_IN), d)
    ev_conv_w = nrm((n_even, CONV_W, LRU_WIDTH), CONV_W)
    ev_conv_b = bias((n_even, LRU_WIDTH))
    ev_lru_wa = nrm((n_even, 2, LRU_BLOCKS, LRU_BLOCK, LRU_BLOCK), LRU_BLOCK)
    ev_lru_ba = bias((n_even, 2, LRU_WIDTH))
    ev_lru_wi = nrm((n_even, 2, LRU_BLOCKS, LRU_BLOCK, LRU_BLOCK), LRU_BLOCK)
    ev_lru_bi = bias((n_even, 2, LRU_WIDTH))
    a_init = jax.random.uniform(next(ks), (n_even, 2, LRU_WIDTH), F32, 0.9, 0.999)
    p_init = a_init ** (1.0 / LRU_C)
    ev_lru_lam = jnp.log(p_init) - jnp.log1p(-p_init)
    ev_gla_wg = nrm((n_even, 2, GLA_RANK, GLA_KEY), GLA_RANK)
    ev_gla_bg = bias((n_even, 2, GLA_KEY), 0.5)
    ev_gla_norm = gain((n_even, GLA_DV))
    ev_w_out = nrm((n_even, MIX_WIDTH, d), MIX_WIDTH)
    od_w_qkv = nrm((n_odd, d, QKV_WIDTH), d)
    od_q_norm = gain((n_odd, HEAD_DIM))
    od_k_norm = gain((n_odd, HEAD_DIM))
    od_w_o = nrm((n_odd, ATT_HEADS * HEAD_DIM, d), ATT_HEADS * HEAD_DIM)
    moe_wg = nrm((DEPTH, d, N_GROUPS), d)
    moe_bg = bias((DEPTH, N_GROUPS), 0.01)
    moe_we = nrm((DEPTH, d, N_EXPERTS), d)
    moe_be = bias((DEPTH, N_EXPERTS), 0.01)
    moe_w1 = nrm((DEPTH, N_EXPERTS, d, EXPERT_HIDDEN), d)
    moe_w3 = nrm((DEPTH, N_EXPERTS, d, EXPERT_HIDDEN), d)
    moe_w2 = nrm((DEPTH, N_EXPERTS, EXPERT_HIDDEN, d), EXPERT_HIDDEN)
    return {"x": x, "c": c, "ctx": ctx, "c_ctx": c_ctx, "norm1": norm1, "norm2": norm2,
            "w_ada": w_ada, "b_ada": b_ada, "ev_w_in": ev_w_in, "ev_conv_w": ev_conv_w,
            "ev_conv_b": ev_conv_b, "ev_lru_wa": ev_lru_wa, "ev_lru_ba": ev_lru_ba,
            "ev_lru_wi": ev_lru_wi, "ev_lru_bi": ev_lru_bi, "ev_lru_lam": ev_lru_lam,
            "ev_gla_wg": ev_gla_wg, "ev_gla_bg": ev_gla_bg, "ev_gla_norm": ev_gla_norm,
            "ev_w_out": ev_w_out, "od_w_qkv": od_w_qkv, "od_q_norm": od_q_norm,
            "od_k_norm": od_k_norm, "od_w_o": od_w_o, "moe_wg": moe_wg, "moe_bg": moe_bg,
            "moe_we": moe_we, "moe_be": moe_be, "moe_w1": moe_w1, "moe_w3": moe_w3,
            "moe_w2": moe_w2}


def reference(x, c, ctx, c_ctx, norm1, norm2, w_ada, b_ada, ev_w_in, ev_conv_w, ev_conv_b,
              ev_lru_wa, ev_lru_ba, ev_lru_wi, ev_lru_bi, ev_lru_lam, ev_gla_wg, ev_gla_bg,
              ev_gla_norm, ev_w_out, od_w_qkv, od_q_norm, od_k_norm, od_w_o, moe_wg, moe_bg,
              moe_we, moe_be, moe_w1, moe_w3, moe_w2):
    ROWS = x.shape[1] // GRID_W
    cos, sin = rope_tables(ROWS)
    xl, xc = x, ctx
    for i in range(DEPTH):
        last = i == DEPTH - 1
        j = i // 2
        sh1l, sc1l, g1l, sh2l, sc2l, g2l = adaln(c, w_ada[i], b_ada[i])
        sh1c, sc1c, g1c, sh2c, sc2c, g2c = adaln(c_ctx[None], w_ada[i], b_ada[i])
        hl = modulate(rmsnorm(xl, norm1[i]), sh1l, sc1l)
        hc = modulate(rmsnorm(xc, norm1[i]), sh1c, sc1c)
        if i % 2 == 0:
            yc, yl = rglru_gla_mixer(hc, hl, ev_w_in[j], ev_conv_w[j], ev_conv_b[j], ev_lru_wa[j],
                                     ev_lru_ba[j], ev_lru_wi[j], ev_lru_bi[j], ev_lru_lam[j],
                                     ev_gla_wg[j], ev_gla_bg[j], ev_gla_norm[j], ev_w_out[j],
                                     not last)
        else:
            yc, yl = gqa_mixer(hc, hl, od_w_qkv[j], od_q_norm[j], od_k_norm[j], od_w_o[j],
                               cos, sin, not last)
        xl = xl + g1l * yl
        hl2 = modulate(rmsnorm(xl, norm2[i]), sh2l, sc2l)
        xl = xl + g2l * hier_moe(hl2, moe_wg[i], moe_bg[i], moe_we[i], moe_be[i],
                                 moe_w1[i], moe_w3[i], moe_w2[i])
        if not last:
            xc = xc + g1c * yc
            hc2 = modulate(rmsnorm(xc, norm2[i]), sh2c, sc2c)
            xc = xc + g2c * hier_moe(hc2, moe_wg[i], moe_bg[i], moe_we[i], moe_be[i],
                                     moe_w1[i], moe_w3[i], moe_w2[i])
    return xl
```

```python
import numpy as np
from contextlib import ExitStack
import concourse.bass as bass
import concourse.mybir as mybir
from concourse.bass_utils import run_bass_kernel_spmd
from concourse.alu_op_type import AluOpType as ALU

AF = mybir.ActivationFunctionType
AX = mybir.AxisListType
F32 = mybir.dt.float32
BF16 = mybir.dt.bfloat16
I32 = mybir.dt.int32

D = 1024
KC = 8
EPS = 1e-6
NEXP = 16
HID = 512
BIG = 1.0e30


class Buf:
    __slots__ = ("t", "w", "r", "name")

    def __init__(self, t, name=""):
        self.t = t
        self.w = None
        self.r = {}
        self.name = name

    def __getitem__(self, idx):
        return self.t[idx]


class KB:
    ND = 40

    def __init__(self, nc, es):
        self.nc = nc
        self.es = es
        self.E = {"pe": nc.tensor, "act": nc.scalar, "dve": nc.vector, "pool": nc.gpsimd, "sp": nc.sync}
        self.csem = {e: es.enter_context(nc.semaphore("c_" + e)) for e in ("pe", "act", "dve", "pool")}
        self.ccnt = {e: 0 for e in self.csem}
        self.dsem = [es.enter_context(nc.semaphore("d%d" % i)) for i in range(self.ND)]
        self.dval = [0] * self.ND
        self.di = 0
        self.seen = {e: {} for e in self.E}
        self.nbuf = 0
        self.ninst = 0
        self.phase = None
        import os as _os
        self.maxops = int(_os.environ.get("KB_MAXOPS", "1000000000"))
        self.skipped = False
        self.pend_inc = {}
        self.rec = None
        self._open = False

    def sb(self, shape, dt, name=None):
        self.nbuf += 1
        name = (name or "b") + "_%d" % self.nbuf
        stack = self.phase if self.phase is not None else self.es
        return Buf(stack.enter_context(self.nc.sbuf_tensor(name, list(shape), dt)), name)

    def push(self):
        assert self.phase is None
        self.phase = ExitStack()

    def pop(self):
        self.barrier()
        self.phase.close()
        self.phase = None

    def barrier(self):
        for eng in self.E:
            for e2 in self.csem:
                v = self.ccnt[e2]
                if v > self.seen[eng].get(e2, 0):
                    self.E[eng].wait_ge(self.csem[e2], v)
                    self.seen[eng][e2] = v
            for i in range(self.ND):
                v = self.dval[i]
                if v > self.seen[eng].get(("d", i), 0):
                    self.E[eng].wait_ge(self.dsem[i], v)
                    self.seen[eng][("d", i)] = v

    def ps(self, shape, dt, name=None):
        self.nbuf += 1
        name = (name or "p") + "_%d" % self.nbuf
        return Buf(self.es.enter_context(self.nc.psum_tensor(name, list(shape), dt)), name)

    def dram(self, name, shape, dt, kind="Internal"):
        return Buf(self.nc.dram_tensor(name, list(shape), dt, kind=kind).ap(), name)

    def semobj(self, sk):
        return self.csem[sk] if isinstance(sk, str) else self.dsem[sk[1]]

    def _deps(self, eng, reads, writes):
        need = {}
        for b in reads:
            if b.w is not None:
                need[b.w[0]] = max(need.get(b.w[0], 0), b.w[1])
        for b in writes:
            if b.w is not None:
                need[b.w[0]] = max(need.get(b.w[0], 0), b.w[1])
            for sk, v in b.r.items():
                need[sk] = max(need.get(sk, 0), v)
        for sk, v in need.items():
            if eng == "pe" and sk == "pe":
                continue
            if self.seen[eng].get(sk, 0) >= v:
                continue
            self.E[eng].wait_ge(self.semobj(sk), v)
            self.seen[eng][sk] = v

    def _done(self, tok, reads, writes):
        for b in reads:
            b.r[tok[0]] = max(b.r.get(tok[0], 0), tok[1])
        for b in writes:
            b.w = tok
            b.r = {}

    def replay(self, lst, n):
        k = 0
        while lst and (k < n or self._open):
            it = lst.pop(0)
            if it[0] == "op":
                _, eng, fn, reads, writes, inc = it
                self._open = not inc
                self.op(eng, fn, reads, writes, inc)
            else:
                _, q, out, in_, reads, writes, kw = it
                self._open = False
                self.dma(q, out, in_, reads, writes, **kw)
            k += 1

    def op(self, eng, fn, reads=(), writes=(), inc=True):
        if self.rec is not None:
            self.rec.append(("op", eng, fn, tuple(reads), tuple(writes), inc))
            return
        if self.ninst >= self.maxops:
            if not self.skipped:
                self.skipped = True
                print("KB: first skipped op", eng, [b.name for b in reads], [b.name for b in writes])
            if inc and self.pend_inc.get(eng):
                pass
            return
        self._deps(eng, reads, writes)
        ins = fn(self.E[eng])
        self.ninst += 1
        if inc:
            self.ccnt[eng] += 1
            ins.then_inc(self.csem[eng], 1)
            tok = (eng, self.ccnt[eng])
        else:
            tok = (eng, self.ccnt[eng] + 1)
        self._done(tok, reads, writes)

    def dma(self, q, out, in_, reads=(), writes=(), **kw):
        if self.rec is not None:
            self.rec.append(("dma", q, out, in_, tuple(reads), tuple(writes), kw))
            return
        if self.ninst >= self.maxops:
            if not self.skipped:
                self.skipped = True
                print("KB: first skipped dma", q, [b.name for b in reads], [b.name for b in writes])
            return
        i = self.di % self.ND
        self.di += 1
        self._deps(q, reads, writes)
        sk = ("d", i)
        if self.dval[i] > 0 and self.seen[q].get(sk, 0) < self.dval[i]:
            self.E[q].wait_ge(self.dsem[i], self.dval[i])
            self.seen[q][sk] = self.dval[i]
        self.E[q].dma_start(out=out, in_=in_, **kw).then_inc(self.dsem[i], 16)
        self.ninst += 1
        self.dval[i] += 16
        self._done((sk, self.dval[i]), reads, writes)

    def wait_all(self, eng, bufs):
        self._deps(eng, bufs, ())


def bcast(ap, dims):
    raise NotImplementedError


def mkap(t_ap, pat, off=0):
    return bass.AP(t_ap.tensor, t_ap.offset + off, [list(t_ap.ap[0])] + [list(p) for p in pat])


def run_pipeline(gens, depth):
    active = []
    it = iter(gens)
    done = False
    while True:
        if not done and len(active) < depth:
            try:
                active.append(next(it))
            except StopIteration:
                done = True
        if not active:
            if done:
                break
            continue
        for g in list(active):
            try:
                next(g)
            except StopIteration:
                active.remove(g)


def token_groups(TC, TL, g=512):
    out = []
    t = 0
    while t < TC:
        n = min(g, TC - t)
        out.append((t, n))
        t += n
    while t < TC + TL:
        n = min(g, TC + TL - t)
        out.append((t, n))
        t += n
    return out


def build(cfg):
    NS, TC, TL = cfg["NS"], cfg["TC"], cfg["TL"]
    DBG = cfg.get("DBG", False)
    STOP = cfg.get("STOP", "end")
    T = TC + TL
    NT = T // 128
    NTC = TC // 128
    NV = NS + 1
    nc = bass.Bass("TRN2", target_bir_lowering=False)
    es = ExitStack()
    kb = KB(nc, es)
    okind = "ExternalOutput" if DBG else "Internal"

    def din(name, shape, dt=F32):
        return Buf(nc.dram_tensor(name, list(shape), dt, kind="ExternalInput").ap(), name)

    x_d = din("x", [NS, TL, D])
    ctx_d = din("ctx", [NS, TC, D])
    cv_d = din("cvec", [128, KC, NV])
    n1_d = din("norm1c", [2, 128, KC])
    n2_d = din("norm2c", [2, 128, KC])
    wada_d = din("w_ada", [2, 128, KC, 6 * D])
    bada_d = din("b_adac", [2, 128, 48])
    win_d = din("w_in", [128, KC, 2592])
    convw_d = din("conv_w", [128, 4, 4])
    convb_d = din("conv_b", [128, 4])
    lwa_d = din("lru_wa", [2, 4, 128, 128])
    lwi_d = din("lru_wi", [2, 4, 128, 128])
    lba_d = din("lru_ba", [128, 2, 4])
    lbi_d = din("lru_bi", [128, 2, 4])
    llam_d = din("lru_lam", [128, 2, 4])
    gwg_d = din("gla_wg", [2, 16, 256])
    gbg_d = din("gla_bg", [128, 2, 2])
    gnorm_d = din("gla_norm", [1, 128])
    wout_d = din("w_out", [128, KC, D])
    wqkv_d = din("w_qkv", [128, KC, 1536])
    qn_d = din("q_norm", [1, 128])
    kn_d = din("k_norm", [1, 128])
    wo_d = din("w_o", [128, KC, D])
    wr_d = din("moe_wr", [2, 128, KC, 20])
    br_d = din("moe_br", [2, 1, 20])
    w1_d = din("moe_w1", [2, NEXP, 128, KC, HID])
    w3_d = din("moe_w3", [2, NEXP, 128, KC, HID])
    w2_d = din("moe_w2", [2, NEXP, 128, 4, D])
    ident_d = din("ident", [128, 128])
    maskf_d = din("maskf", [128, 128])
    maskb_d = din("maskb", [128, 128])
    cos_d = din("rope_cos", [TL, 64])
    sin_d = din("rope_sin", [TL, 64])
    out_d = Buf(nc.dram_tensor("out", [NS, TL, D], F32, kind="ExternalOutput").ap(), "out")

    xaT_d = [kb.dram("xaT%d" % s, [512, T], F32, okind) for s in range(NS)]
    gaT_d = [kb.dram("gaT%d" % s, [512, T], F32, okind) for s in range(NS)]
    qT_d = [kb.dram("qT%d" % s, [256, T], F32, okind) for s in range(NS)]
    kT_d = [kb.dram("kT%d" % s, [256, T], F32, okind) for s in range(NS)]
    lrT_d = [kb.dram("lrT%d" % s, [2, 16, T], F32, okind) for s in range(NS)]
    v_d = [kb.dram("vtok%d" % s, [T, 512], BF16, okind) for s in range(NS)]
    r_d = [kb.dram("rtok%d" % s, [T, 512], F32, okind) for s in range(NS)]
    of_d = [kb.dram("ofirst%d" % s, [T, 512], F32, okind) for s in range(NS)]
    mixT_d = [kb.dram("mixT%d" % s, [D, T], BF16, okind) for s in range(NS)]
    x1_d = [kb.dram("x1_%d" % s, [T, D], F32, okind) for s in range(NS)]
    q1T_d = [kb.dram("q1T%d" % s, [128, 8, TL], BF16, okind) for s in range(NS)]
    k1T_d = [kb.dram("k1T%d" % s, [128, 2, T], BF16, okind) for s in range(NS)]
    v1_d = [kb.dram("v1e%d" % s, [T, 2, 130], BF16, okind) for s in range(NS)]
    attT_d = [kb.dram("attT%d" % s, [D, TL], BF16, okind) for s in range(NS)]

    PSB = [kb.ps([128, 512], F32, "bank") for _ in range(8)]
    pstate = {"i": 0}

    pstate["set"] = list(range(8))

    def psum():
        st_ = pstate["set"]
        b = PSB[st_[pstate["i"] % len(st_)]]
        pstate["i"] += 1
        return b

    ident = kb.sb([128, 128], F32, "ident")
    identb = kb.sb([128, 128], BF16, "identb")
    ones = kb.sb([128, 128], F32, "ones")
    kb.dma("sp", ident[:], ident_d[:], [ident_d], [ident])
    kb.op("dve", lambda e: e.tensor_copy(out=identb[:], in_=ident[:]), [ident], [identb])
    kb.op("dve", lambda e: e.memset(ones[:], 1.0), [], [ones])

    rr = {"i": 0}

    def evac_eng():
        rr["i"] += 1
        return "act" if rr["i"] % 2 == 0 else "dve"

    def copy(eng, out_b, out_ap, in_b, in_ap):
        if eng == "act":
            kb.op("act", lambda e: e.copy(out=out_ap, in_=in_ap), [in_b], [out_b])
        elif eng == "dve":
            kb.op("dve", lambda e: e.tensor_copy(out=out_ap, in_=in_ap), [in_b], [out_b])
        else:
            kb.op("pool", lambda e: e.tensor_copy(out=out_ap, in_=in_ap), [in_b], [out_b])

    cv = kb.sb([128, KC, NV], F32, "cv")
    kb.dma("sp", cv[:], cv_d[:], [cv_d], [cv])
    kb.op("act", lambda e: e.activation(out=cv[:], in_=cv[:], func=AF.Silu), [cv], [cv])
    mT = [kb.sb([128, 48, NV], F32, "mT") for _ in range(2)]
    n1c = kb.sb([128, 2, KC], F32, "n1c")
    n2c = kb.sb([128, 2, KC], F32, "n2c")
    badac = kb.sb([128, 2, 48], F32, "badac")
    for l in range(2):
        kb.dma("sp", n1c[:, l, :], n1_d[l], [n1_d], [n1c])
        kb.dma("sp", n2c[:, l, :], n2_d[l], [n2_d], [n2c])
        kb.dma("sp", badac[:, l, :], bada_d[l], [bada_d], [badac])
    Ac = kb.sb([128, 2, 2, NV, KC], F32, "Ac")
    Bc = kb.sb([128, 2, 2, NV, KC], F32, "Bc")
    diag = kb.sb([128, 128], F32, "diag")
    kb.push()
    wst = [kb.sb([128, KC, 256], F32, "wst") for _ in range(2)]
    wsti = {"i": 0}

    def stage():
        b = wst[wsti["i"] % 2]
        wsti["i"] += 1
        return b

    for l in range(2):
        for g in range(24):
            st = stage()
            kb.dma("sp", st[:], wada_d[l, :, :, g * 256:(g + 1) * 256], [wada_d], [st])
            pb = psum()
            for jj in range(2):
                for kc in range(KC):
                    kb.op("pe", lambda e, jj=jj, kc=kc: e.matmul(
                        pb[:, jj * NV:(jj + 1) * NV], lhsT=st[:, kc, jj * 128:(jj + 1) * 128], rhs=cv[:, kc, :],
                        start=(kc == 0), stop=(kc == KC - 1)), [st, cv], [pb], inc=(kc == KC - 1))
            for jj in range(2):
                ch = g * 2 + jj
                kb.op("dve", lambda e, jj=jj, ch=ch: e.tensor_scalar(
                    out=mT[l][:, ch, :], in0=pb[:, jj * NV:(jj + 1) * NV], scalar1=badac[:, l, ch:ch + 1], scalar2=None,
                    op0=ALU.add), [pb, badac], [mT[l]])
    kb.pop()
    for l in range(2):
        for sub in range(2):
            nrm = n1c if sub == 0 else n2c
            for v in range(NV):
                sh = mT[l][:, sub * 24 + 0:sub * 24 + 8, v]
                sc = mT[l][:, sub * 24 + 8:sub * 24 + 16, v]
                kb.op("dve", lambda e, sc=sc, l=l, sub=sub, v=v, nrm=nrm: e.scalar_tensor_tensor(
                    out=Ac[:, l, sub, v, :], in0=sc, scalar=1.0, in1=nrm[:, l, :], op0=ALU.add, op1=ALU.mult),
                    [mT[l], nrm], [Ac])
                kb.op("dve", lambda e, sh=sh, l=l, sub=sub, v=v: e.tensor_copy(out=Bc[:, l, sub, v, :], in_=sh),
                      [mT[l]], [Bc])

    def make_grow(l, sub, v):
        gb = kb.sb([128, D], F32, "G")
        for half in range(2):
            pb = psum()
            for jj in range(4):
                j = half * 4 + jj
                col = mT[l][:, sub * 24 + 16 + j, v:v + 1]
                kb.op("dve", lambda e, col=col: e.tensor_scalar(
                    out=diag[:], in0=ident[:], scalar1=col, scalar2=None, op0=ALU.mult),
                    [ident, mT[l]], [diag])
                kb.op("pe", lambda e, jj=jj, pb=pb: e.matmul(pb[:, jj * 128:(jj + 1) * 128], lhsT=ones[:], rhs=diag[:],
                                                       start=True, stop=True), [ones, diag], [pb])
            copy("act", gb, gb[:, half * 512:(half + 1) * 512], pb, pb[:])
        return gb

    gdram = {}
    kb.push()
    for l in range(2):
        for sub in range(2):
            for v in range(NV):
                if v == NS and l == 1:
                    continue
                gb = make_grow(l, sub, v)
                gd = kb.dram("gd_%d_%d_%d" % (l, sub, v), [128, D], F32)
                gdram[(l, sub, v)] = gd
                kb.dma("pool", gd[:], gb[:], [gb], [gd])
    kb.pop()

    stat = [kb.sb([128, 4], F32, "stat") for _ in range(4)]
    xn_b = [kb.sb([128, D], F32, "xn") for _ in range(2)]
    nm = {"i": 0}

    def norm_mod_T(xb, x_ap, l, sub, v, hT, hT_ap_fn, h32=None):
        i = nm["i"]
        nm["i"] += 1
        stt = stat[i % 4]
        xn = xn_b[i % 2]
        kb.op("act", lambda e: e.activation(out=xn[:], in_=x_ap, func=AF.Square, accum_out=stt[:, 0:1]),
              [xb], [xn, stt])
        kb.op("dve", lambda e: e.tensor_scalar(out=stt[:, 1:2], in0=stt[:, 0:1], scalar1=1.0 / D, scalar2=EPS,
                                                op0=ALU.mult, op1=ALU.add), [stt], [stt])
        kb.op("act", lambda e: e.activation(out=stt[:, 2:3], in_=stt[:, 1:2], func=AF.Sqrt), [stt], [stt])
        kb.op("dve", lambda e: e.reciprocal(out=stt[:, 3:4], in_=stt[:, 2:3]), [stt], [stt])
        kb.op("dve", lambda e: e.tensor_scalar(out=xn[:], in0=x_ap, scalar1=stt[:, 3:4], scalar2=None, op0=ALU.mult),
              [xb, stt], [xn])
        for half in range(2):
            pb = psum()
            for jj in range(4):
                j = half * 4 + jj
                kb.op("pe", lambda e, jj=jj, j=j, pb=pb: e.transpose(out=pb[:, jj * 128:(jj + 1) * 128],
                                                                     in_=xn[:, j * 128:(j + 1) * 128], identity=ident[:]),
                      [xn, ident], [pb])
            for jj in range(4):
                j = half * 4 + jj
                kb.op("act", lambda e, jj=jj, j=j, pb=pb: e.activation(
                    out=hT_ap_fn(j), in_=pb[:, jj * 128:(jj + 1) * 128], func=AF.Identity,
                    scale=Ac[:, l, sub, v, j:j + 1], bias=Bc[:, l, sub, v, j:j + 1]), [pb, Ac, Bc], [hT])
                if h32 is not None:
                    kb.op("act", lambda e, jj=jj, j=j, pb=pb: e.activation(
                        out=h32[:, j, :], in_=pb[:, jj * 128:(jj + 1) * 128], func=AF.Identity,
                        scale=Ac[:, l, sub, v, j:j + 1], bias=Bc[:, l, sub, v, j:j + 1]), [pb, Ac, Bc], [h32])

    def tile_src(s, t0):
        if t0 < TC:
            return ctx_d, ctx_d[s, t0:t0 + 128, :]
        return x_d, x_d[s, t0 - TC:t0 - TC + 128, :]

    def load_w_bf16(dst, dst_ap_fn, src_b, src_ap_fn, ncols, step=512):
        for c0 in range(0, ncols, step):
            n = min(step, ncols - c0)
            kb.dma("pool", dst_ap_fn(c0, n), src_ap_fn(c0, n), [src_b], [dst])

    groups = token_groups(TC, TL)
    kb.push()
    winb = kb.sb([128, KC, 2592], BF16, "winb")
    load_w_bf16(winb, lambda c0, n: winb[:, :, c0:c0 + n], win_d, lambda c0, n: win_d[:, :, c0:c0 + n], 2592)
    xt_b = [kb.sb([128, D], F32, "xt") for _ in range(3)]
    hTg = [kb.sb([128, KC, 512], BF16, "hTg") for _ in range(3)]
    fst = [kb.sb([128, 512], F32, "fst") for _ in range(4)]
    vst = [kb.sb([128, 512], BF16, "vst") for _ in range(2)]
    cnt = {"x": 0, "g": 0, "f": 0, "v": 0}
    fchunks = []
    for cc in range(4):
        fchunks.append((cc * 128, 128, xaT_d, cc * 128))
    for cc in range(4):
        fchunks.append((512 + cc * 128, 128, gaT_d, cc * 128))
    for cc in range(2):
        fchunks.append((1024 + cc * 128, 128, qT_d, cc * 128))
    for cc in range(2):
        fchunks.append((1280 + cc * 128, 128, kT_d, cc * 128))
    def p0_task(s, t0, n):
            v = NS if t0 < TC else s
            hT = hTg[cnt["g"] % 3]
            cnt["g"] += 1
            for i in range(n // 128):
                xt = xt_b[cnt["x"] % 3]
                cnt["x"] += 1
                sb_, sap = tile_src(s, t0 + i * 128)
                kb.dma("sp", xt[:], sap, [sb_], [xt])
                norm_mod_T(xt, xt[:], 0, 0, v, hT, lambda j, i=i, hT=hT: hT[:, j, i * 128:(i + 1) * 128])
            yield
            for (c0, M, dst, r0) in fchunks:
                pb = psum()
                for kc in range(KC):
                    kb.op("pe", lambda e, kc=kc, c0=c0, M=M, pb=pb: e.matmul(
                        pb[0:M, 0:n], lhsT=winb[:, kc, c0:c0 + M], rhs=hT[:, kc, 0:n],
                        start=(kc == 0), stop=(kc == KC - 1)), [winb, hT], [pb], inc=(kc == KC - 1))
                st = fst[cnt["f"] % 4]
                cnt["f"] += 1
                copy(evac_eng(), st, st[0:M, 0:n], pb, pb[0:M, 0:n])
                kb.dma("pool", dst[s][r0:r0 + M, t0:t0 + n], st[0:M, 0:n], [st], [dst[s]])
            for dd in range(2):
                pb = psum()
                c0 = 2560 + dd * 16
                for kc in range(KC):
                    kb.op("pe", lambda e, kc=kc, c0=c0, pb=pb: e.matmul(
                        pb[0:16, 0:n], lhsT=winb[:, kc, c0:c0 + 16], rhs=hT[:, kc, 0:n],
                        start=(kc == 0), stop=(kc == KC - 1)), [winb, hT], [pb], inc=(kc == KC - 1))
                st = fst[cnt["f"] % 4]
                cnt["f"] += 1
                copy(evac_eng(), st, st[0:16, 0:n], pb, pb[0:16, 0:n])
                kb.dma("pool", lrT_d[s][dd, :, t0:t0 + n], st[0:16, 0:n], [st], [lrT_d[s]])
            yield
            for i in range(n // 128):
                tt = t0 + i * 128
                for which in range(2):
                    c0 = 1536 + which * 512
                    pb = psum()
                    for kc in range(KC):
                        kb.op("pe", lambda e, kc=kc, c0=c0, pb=pb, i=i: e.matmul(
                            pb[:, :], lhsT=hT[:, kc, i * 128:(i + 1) * 128], rhs=winb[:, kc, c0:c0 + 512],
                            start=(kc == 0), stop=(kc == KC - 1)), [winb, hT], [pb], inc=(kc == KC - 1))
                    if which == 0:
                        st = vst[cnt["v"] % 2]
                        cnt["v"] += 1
                        copy(evac_eng(), st, st[:], pb, pb[:])
                        kb.dma("pool", v_d[s][tt:tt + 128, :], st[:], [st], [v_d[s]])
                    else:
                        st = fst[cnt["f"] % 4]
                        cnt["f"] += 1
                        copy(evac_eng(), st, st[:], pb, pb[:])
                        kb.dma("pool", r_d[s][tt:tt + 128, :], st[:], [st], [r_d[s]])

    run_pipeline((p0_task(s, t0, n) for s in range(NS) for (t0, n) in groups), 3)

    def finish():
        print("KB: ninst at finish", kb.ninst, "stop", STOP)
        if kb.phase is not None:
            kb.pop()
        else:
            kb.barrier()
        es.close()
        return nc

    kb.pop()
    if STOP == "P0":
        return finish()

    def ps16(pb):
        return pb[:].bitcast(BF16)

    kb.push()
    cw = kb.sb([128, 4, 4], F32, "cw")
    cb = kb.sb([128, 4], F32, "cb")
    lba = kb.sb([128, 2, 4], F32, "lba")
    lbi = kb.sb([128, 2, 4], F32, "lbi")
    lam = kb.sb([128, 2, 4], F32, "lam")
    cl1 = kb.sb([128, 2, 4], F32, "cl1")
    cl2 = kb.sb([128, 2, 4], F32, "cl2")
    kb.dma("sp", cw[:], convw_d[:], [convw_d], [cw])
    kb.dma("sp", cb[:], convb_d[:], [convb_d], [cb])
    kb.dma("sp", lba[:], lba_d[:], [lba_d], [lba])
    kb.dma("sp", lbi[:], lbi_d[:], [lbi_d], [lbi])
    kb.dma("sp", lam[:], llam_d[:], [llam_d], [lam])
    kb.op("act", lambda e: e.activation(out=lam[:], in_=lam[:], func=AF.Exp, scale=-1.0), [lam], [lam])
    kb.op("act", lambda e: e.activation(out=lam[:], in_=lam[:], func=AF.Ln, bias=1.0), [lam], [lam])
    kb.op("dve", lambda e: e.tensor_scalar(out=cl1[:], in0=lam[:], scalar1=-8.0, scalar2=None, op0=ALU.mult), [lam], [cl1])
    kb.op("dve", lambda e: e.tensor_scalar(out=cl2[:], in0=lam[:], scalar1=-16.0, scalar2=None, op0=ALU.mult), [lam], [cl2])
    lw = kb.sb([128, 2, 8, 128], BF16, "lw")
    lst = [kb.sb([128, KC, 128], F32, "lst") for _ in range(2)]
    for ai, wd in enumerate((lwa_d, lwi_d)):
        st = lst[ai]
        stv = st[:].rearrange("p a b -> p (a b)")[:, 0:1024].rearrange("p (a b) -> p a b", b=128)
        kb.dma("sp", stv, wd[:].rearrange("d c k n -> k (d c) n"), [wd], [st])
        kb.op("pool", lambda e, ai=ai, stv=stv: e.tensor_copy(out=lw[:, ai, :, :], in_=stv), [st], [lw])
    BX = kb.sb([128, T], F32, "BX")
    BU = kb.sb([128, T], F32, "BU")
    BUB = kb.sb([128, T], BF16, "BUB")
    BR = kb.sb([128, T], F32, "BR")
    BI = kb.sb([128, T], F32, "BI")
    BHS = kb.sb([128, T], F32, "BHS")
    BH = kb.sb([128, T], F32, "BH")
    BY = kb.sb([128, T], BF16, "BY")
    g512 = [(t0, min(512, T - t0)) for t0 in range(0, T, 512)]
    segs = [(0, TC), (TC, T)]
    for s in range(NS):
        for cc in range(4):
            kb.dma("sp", BX[:], xaT_d[s][cc * 128:(cc + 1) * 128, :], [xaT_d[s]], [BX])
            kb.op("dve", lambda e, cc=cc: e.tensor_scalar(out=BU[:], in0=BX[:], scalar1=cw[:, cc, 2:3],
                                                          scalar2=cb[:, cc:cc + 1], op0=ALU.mult, op1=ALU.add),
                  [BX, cw, cb], [BU])
            for (s0, s1) in segs:
                for j in (0, 1, 3):
                    o = j - 2
                    lo = max(s0, s0 - o)
                    hi = min(s1, s1 - o)
                    kb.op("dve", lambda e, cc=cc, j=j, lo=lo, hi=hi, o=o: e.scalar_tensor_tensor(
                        out=BU[:, lo:hi], in0=BX[:, lo + o:hi + o], scalar=cw[:, cc, j:j + 1], in1=BU[:, lo:hi],
                        op0=ALU.mult, op1=ALU.add), [BX, cw, BU], [BU])
            kb.op("act", lambda e: e.copy(out=BUB[:], in_=BU[:]), [BU], [BUB])
            for d in range(2):
                for (t0, n) in g512:
                    for ai, dst, bb in ((0, BR, lba), (1, BI, lbi)):
                        pb = psum()
                        kb.op("pe", lambda e, ai=ai, pb=pb, t0=t0, n=n, d=d, cc=cc: e.matmul(
                            pb[:, 0:n], lhsT=lw[:, ai, d * 4 + cc, :], rhs=BUB[:, t0:t0 + n], start=True, stop=True),
                            [lw, BUB], [pb])
                        kb.op("act", lambda e, pb=pb, dst=dst, bb=bb, t0=t0, n=n, d=d, cc=cc: e.activation(
                            out=dst[:, t0:t0 + n], in_=pb[:, 0:n], func=AF.Sigmoid, bias=bb[:, d, cc:cc + 1]),
                            [pb, bb], [dst])
                kb.op("act", lambda e, d=d, cc=cc: e.activation(out=BX[:], in_=BR[:], func=AF.Exp,
                                                                scale=cl1[:, d, cc:cc + 1]), [BR, cl1], [BX])
                kb.op("act", lambda e, d=d, cc=cc: e.activation(out=BR[:], in_=BR[:], func=AF.Exp,
                                                                scale=cl2[:, d, cc:cc + 1]), [BR, cl2], [BR])
                kb.op("act", lambda e: e.activation(out=BR[:], in_=BR[:], func=AF.Sqrt, scale=-1.0, bias=1.0),
                      [BR], [BR])
                kb.op("dve", lambda e: e.tensor_tensor(out=BI[:], in0=BI[:], in1=BR[:], op=ALU.mult), [BI, BR], [BI])
                kb.op("pool", lambda e: e.tensor_tensor(out=BI[:], in0=BI[:], in1=BU[:], op=ALU.mult), [BI, BU], [BI])
                H = BHS if d == 0 else BH
                if d == 0:
                    kb.op("dve", lambda e, H=H: e.tensor_tensor_scan(
                        out=H[:, 0:TC], data0=BX[:, 0:TC], data1=BI[:, 0:TC], initial=0.0, op0=ALU.mult, op1=ALU.add),
                        [BX, BI], [H])
                    kb.op("dve", lambda e, H=H: e.tensor_tensor_scan(
                        out=H[:, TC:T], data0=BX[:, TC:T], data1=BI[:, TC:T], initial=H[:, TC - 1:TC],
                        op0=ALU.mult, op1=ALU.add), [BX, BI, H], [H])
                else:
                    kb.op("dve", lambda e, H=H: e.tensor_tensor_scan(
                        out=H[:, 0:TC][:, ::-1], data0=BX[:, 0:TC][:, ::-1], data1=BI[:, 0:TC][:, ::-1], initial=0.0,
                        op0=ALU.mult, op1=ALU.add), [BX, BI], [H])
                    kb.op("dve", lambda e, H=H: e.tensor_tensor_scan(
                        out=H[:, TC:T][:, ::-1], data0=BX[:, TC:T][:, ::-1], data1=BI[:, TC:T][:, ::-1],
                        initial=H[:, 0:1], op0=ALU.mult, op1=ALU.add), [BX, BI, H], [H])
            kb.dma("sp", BR[:], gaT_d[s][cc * 128:(cc + 1) * 128, :], [gaT_d[s]], [BR])
            kb.op("act", lambda e: e.activation(out=BR[:], in_=BR[:], func=AF.Gelu_apprx_tanh), [BR], [BR])
            kb.op("pool", lambda e: e.tensor_tensor(out=BHS[:], in0=BHS[:], in1=BH[:], op=ALU.add), [BHS, BH], [BHS])
            kb.op("dve", lambda e: e.tensor_tensor(out=BY[:], in0=BHS[:], in1=BR[:], op=ALU.mult), [BHS, BR], [BY])
            kb.dma("pool", mixT_d[s][cc * 128:(cc + 1) * 128, :], BY[:], [BY], [mixT_d[s]])
    kb.pop()
    if STOP == "M0a":
        return finish()

    kb.push()
    maskf = kb.sb([128, 128], F32, "maskf")
    maskb = kb.sb([128, 128], F32, "maskb")
    kb.dma("sp", maskf[:], maskf_d[:], [maskf_d], [maskf])
    kb.dma("sp", maskb[:], maskb_d[:], [maskb_d], [maskb])
    masks = (maskf, maskb)
    rm = [kb.sb([128, 512], F32, "rm") for _ in range(2)]
    for d in range(2):
        kb.op("pool", lambda e, d=d: e.memset(rm[d][:], 1.0), [], [rm[d]])
        off = 0 if d == 0 else 127
        kb.op("pool", lambda e, d=d, off=off: e.memset(rm[d][:, off::128], 0.0), [], [rm[d]])
    wg = kb.sb([16, 2, 256], F32, "wg")
    kb.dma("sp", wg[:], gwg_d[:].rearrange("d k n -> k d n"), [gwg_d], [wg])
    nbg = kb.sb([128, 2, 2], F32, "nbg")
    kb.dma("sp", nbg[:], gbg_d[:], [gbg_d], [nbg])
    kb.op("dve", lambda e: e.tensor_scalar(out=nbg[:], in0=nbg[:], scalar1=-1.0, scalar2=None, op0=ALU.mult), [nbg], [nbg])
    gnb = kb.sb([128, 128], F32, "gnb")
    kb.dma("sp", gnb[:], bass.AP(gnorm_d[:].tensor, 0, [[0, 128], [1, 128]]), [gnorm_d], [gnb])
    vsb = kb.sb([128, NT, 512], BF16, "vsb")
    qg = [[kb.sb([128, T], BF16, "qg") for hp in range(2)] for d in range(2)]
    kg = [[kb.sb([128, T], BF16, "kg") for hp in range(2)] for d in range(2)]
    dec = [[kb.sb([128, NT], F32, "dec") for hp in range(2)] for d in range(2)]
    Sf = [[kb.sb([128, 256], F32, "Sf") for hp in range(2)] for d in range(2)]
    Sb = [[kb.sb([128, 256], BF16, "Sb") for hp in range(2)] for d in range(2)]
    kgm = [kb.sb([128, 128], BF16, "kgm") for _ in range(8)]
    mcol = kb.sb([128, 2], F32, "mcol")
    bm = kb.sb([128, 256], F32, "bm")
    kb.op("pool", lambda e: e.memset(mcol[:], 0.0), [], [mcol])
    kb.op("pool", lambda e: e.memset(mcol[0:64, 0:1], 1.0), [], [mcol])
    kb.op("pool", lambda e: e.memset(mcol[64:128, 1:2], 1.0), [], [mcol])
    kb.op("pool", lambda e: e.memset(bm[:], 0.0), [], [bm])
    kb.op("pool", lambda e: e.memset(bm[0:64, 0:128], 1.0), [], [bm])
    kb.op("pool", lambda e: e.memset(bm[64:128, 128:256], 1.0), [], [bm])
    qgt = [kb.sb([128, 512], F32, "qgt") for _ in range(2)]
    kgt = [kb.sb([128, 512], F32, "kgt") for _ in range(2)]
    lrg = [kb.sb([16, 2, 512], F32, "lrg") for _ in range(2)]
    zt = [kb.sb([128, 512], F32, "zt") for _ in range(2)]
    Gt = [kb.sb([128, 512], F32, "Gt") for _ in range(2)]
    kdT = [kb.sb([128, 128], BF16, "kdT") for _ in range(4)]
    kdtok = [kb.sb([128, 256], BF16, "kdtok") for _ in range(2)]
    attsb = [kb.sb([128, 4, 128], BF16, "attsb") for _ in range(2)]
    ost = [kb.sb([128, 512], F32, "ost") for _ in range(2)]
    ofl = [kb.sb([128, 512], F32, "ofl") for _ in range(2)]
    rtl = [kb.sb([128, 512], F32, "rtl") for _ in range(2)]
    osq = kb.sb([128, 512], F32, "osq")
    gst = [kb.sb([128, 8], F32, "gst") for _ in range(2)]
    ybb = [kb.sb([128, 512], BF16, "ybb") for _ in range(2)]
    ybT = [kb.sb([128, 4, 128], BF16, "ybT") for _ in range(2)]
    gc = {"g": 0, "k": 0, "c": 0, "f": 0}
    for s in range(NS):
        kb.dma("sp", vsb[:], v_d[s][:].rearrange("(n p) c -> p n c", p=128), [v_d[s]], [vsb])
        for (t0, n) in g512:
            i = gc["g"] % 2
            gc["g"] += 1
            kb.dma("sp", lrg[i][:, :, 0:n], lrT_d[s][:, :, t0:t0 + n].rearrange("d k t -> k d t"), [lrT_d[s]], [lrg[i]])
            for hp in range(2):
                j = gc["k"] % 2
                gc["k"] += 1
                kb.dma("sp", qgt[j][:, 0:n], qT_d[s][hp * 128:(hp + 1) * 128, t0:t0 + n], [qT_d[s]], [qgt[j]])
                kb.dma("sp", kgt[j][:, 0:n], kT_d[s][hp * 128:(hp + 1) * 128, t0:t0 + n], [kT_d[s]], [kgt[j]])
                for d in range(2):
                    z = zt[d]
                    G = Gt[d]
                    pb = psum()
                    kb.op("pe", lambda e, pb=pb, d=d, hp=hp, i=i, n=n: e.matmul(
                        pb[:, 0:n], lhsT=wg[:, d, hp * 128:(hp + 1) * 128], rhs=lrg[i][:, d, 0:n], start=True, stop=True),
                        [wg, lrg[i]], [pb])
                    kb.op("act", lambda e, pb=pb, z=z, d=d, hp=hp, n=n: e.activation(
                        out=z[:, 0:n], in_=pb[:, 0:n], func=AF.Exp, scale=-1.0, bias=nbg[:, d, hp:hp + 1]), [pb, nbg], [z])
                    kb.op("act", lambda e, z=z, n=n: e.activation(out=z[:, 0:n], in_=z[:, 0:n], func=AF.Ln, bias=1.0),
                          [z], [z])
                    if d == 0:
                        kb.op("dve", lambda e, z=z, G=G, n=n: e.tensor_tensor_scan(
                            out=G[:, 0:n], data0=rm[0][:, 0:n], data1=z[:, 0:n], initial=0.0, op0=ALU.mult, op1=ALU.add),
                            [rm[0], z], [G])
                    else:
                        kb.op("dve", lambda e, z=z, G=G, n=n: e.tensor_tensor_scan(
                            out=G[:, 0:n][:, ::-1], data0=rm[1][:, 0:n][:, ::-1], data1=z[:, 0:n][:, ::-1], initial=0.0,
                            op0=ALU.mult, op1=ALU.add), [rm[1], z], [G])
                    kb.op("act", lambda e, z=z, G=G, n=n: e.activation(out=z[:, 0:n], in_=G[:, 0:n], func=AF.Exp,
                                                                       scale=-1.0 / 16), [G], [z])
                    kb.op("dve", lambda e, z=z, j=j, d=d, hp=hp, t0=t0, n=n: e.scalar_tensor_tensor(
                        out=qg[d][hp][:, t0:t0 + n], in0=qgt[j][:, 0:n], scalar=0.125, in1=z[:, 0:n],
                        op0=ALU.mult, op1=ALU.mult), [qgt[j], z], [qg[d][hp]])
                    offl = 127 if d == 0 else 0
                    kb.op("pool", lambda e, z=z, d=d, hp=hp, t0=t0, n=n, offl=offl: e.tensor_copy(
                        out=dec[d][hp][:, t0 // 128:(t0 + n) // 128], in_=z[:, offl:n:128]), [z], [dec[d][hp]])
                    kb.op("act", lambda e, G=G, n=n: e.activation(out=G[:, 0:n], in_=G[:, 0:n], func=AF.Exp,
                                                                  scale=1.0 / 16), [G], [G])
                    kb.op("dve", lambda e, G=G, j=j, d=d, hp=hp, t0=t0, n=n: e.tensor_tensor(
                        out=kg[d][hp][:, t0:t0 + n], in0=kgt[j][:, 0:n], in1=G[:, 0:n], op=ALU.mult),
                        [kgt[j], G], [kg[d][hp]])
        print("KB: mark prep-done", kb.ninst)
        order = [list(range(NT)), list(range(NTC - 1, -1, -1)) + list(range(NT - 1, NTC - 1, -1))]
        for d in range(2):
            for hp in range(2):
                kb.op("pool", lambda e, d=d, hp=hp: e.memset(Sf[d][hp][:], 0.0), [], [Sf[d][hp]])
                kb.op("pool", lambda e, d=d, hp=hp: e.memset(Sb[d][hp][:], 0.0), [], [Sb[d][hp]])
        seen_c = set()
        for step in range(NT):
            for d in range(2):
                c = order[d][step]
                cs = slice(c * 128, (c + 1) * 128)
                if step < 2:
                    print("KB: mark step", step, d, kb.ninst)
                ci = gc["c"] % 2
                gc["c"] += 1
                pT = psum()
                pT16 = ps16(pT)
                for hp in range(2):
                    kt = kdT[(gc["c"] * 2 + hp) % 4]
                    kb.op("dve", lambda e, kt=kt, d=d, hp=hp, cs=cs, c=c: e.tensor_scalar(
                        out=kt[:], in0=kg[d][hp][:, cs], scalar1=dec[d][hp][:, c:c + 1], scalar2=None, op0=ALU.mult),
                        [kg[d][hp], dec[d][hp]], [kt])
                    kb.op("pe", lambda e, kt=kt, hp=hp, pT16=pT16: e.transpose(
                        out=pT16[:, hp * 128:(hp + 1) * 128], in_=kt[:], identity=identb[:]), [kt, identb], [pT])
                kdk = kdtok[ci]
                kb.op("act", lambda e, kdk=kdk, pT16=pT16: e.copy(out=kdk[:], in_=pT16[:, 0:256]), [pT], [kdk])
                pA = psum()
                for head in range(4):
                    hp, h = head // 2, head % 2
                    km = kgm[(gc["c"] * 4 + head) % 8]
                    kb.op("dve", lambda e, km=km, d=d, hp=hp, h=h, cs=cs: e.tensor_scalar(
                        out=km[:], in0=kg[d][hp][:, cs], scalar1=mcol[:, h:h + 1], scalar2=None, op0=ALU.mult),
                        [kg[d][hp], mcol], [km])
                    kb.op("pe", lambda e, pA=pA, head=head, hp=hp, km=km, d=d, cs=cs: e.matmul(
                        pA[:, head * 128:(head + 1) * 128], lhsT=km[:], rhs=qg[d][hp][:, cs],
                        start=True, stop=True), [km, qg[d][hp]], [pA])
                asb = attsb[ci]
                mk = masks[d]
                kb.op("dve", lambda e, asb=asb, pA=pA, mk=mk: e.tensor_tensor(
                    out=asb[:], in0=pA[:].rearrange("p (h i) -> p h i", h=4),
                    in1=bass.AP(mk[:].tensor, mk[:].offset, [list(mk[:].ap[0]), [0, 4], [1, 128]]), op=ALU.mult),
                    [pA, mk], [asb])
                pO = psum()
                pD = psum()
                for hp in range(2):
                    pc = slice(hp * 256, (hp + 1) * 256)
                    kb.op("pe", lambda e, pO=pO, hp=hp, pc=pc, d=d, cs=cs: e.matmul(
                        pO[:, pc], lhsT=qg[d][hp][:, cs], rhs=Sb[d][hp][:], start=True, stop=False),
                        [qg[d][hp], Sb[d][hp]], [pO], inc=False)
                    for h in range(2):
                        head = hp * 2 + h
                        hc = slice(head * 128, (head + 1) * 128)
                        kb.op("pe", lambda e, pO=pO, asb=asb, head=head, hc=hc, c=c, h=h: e.matmul(
                            pO[:, hc], lhsT=asb[:, head, :], rhs=vsb[:, c, hc], start=False, stop=(h == 1)),
                            [asb, vsb], [pO], inc=(h == 1))
                for hp in range(2):
                    pc = slice(hp * 256, (hp + 1) * 256)
                    kb.op("pe", lambda e, pD=pD, kdk=kdk, hp=hp, pc=pc, c=c: e.matmul(
                        pD[:, pc], lhsT=kdk[:, hp * 128:(hp + 1) * 128], rhs=vsb[:, c, pc], start=True, stop=True),
                        [kdk, vsb], [pD])
                for hp in range(2):
                    kb.op("dve", lambda e, pD=pD, d=d, hp=hp, c=c: e.scalar_tensor_tensor(
                        out=Sf[d][hp][:], in0=Sf[d][hp][:], scalar=dec[d][hp][:, c:c + 1],
                        in1=pD[:, hp * 256:(hp + 1) * 256], op0=ALU.mult, op1=ALU.add),
                        [Sf[d][hp], dec[d][hp], pD], [Sf[d][hp]])
                    kb.op("dve", lambda e, d=d, hp=hp: e.tensor_tensor(out=Sb[d][hp][:], in0=Sf[d][hp][:], in1=bm[:],
                                                                       op=ALU.mult), [Sf[d][hp], bm], [Sb[d][hp]])
                if c not in seen_c:
                    seen_c.add(c)
                    o1 = ost[ci]
                    kb.op("act", lambda e, o1=o1, pO=pO: e.copy(out=o1[:], in_=pO[:]), [pO], [o1])
                    kb.dma("pool", of_d[s][cs, :], o1[:], [o1], [of_d[s]])
                else:
                    fi = gc["f"] % 2
                    gc["f"] += 1
                    o2 = ofl[fi]
                    rt = rtl[fi]
                    gs = gst[fi]
                    kb.dma("sp", o2[:], of_d[s][cs, :], [of_d[s]], [o2])
                    kb.dma("sp", rt[:], r_d[s][cs, :], [r_d[s]], [rt])
                    kb.op("dve", lambda e, o2=o2, pO=pO: e.tensor_tensor(out=o2[:], in0=pO[:], in1=o2[:], op=ALU.add),
                          [pO, o2], [o2])
                    kb.op("act", lambda e, o2=o2: e.activation(out=osq[:], in_=o2[:], func=AF.Square), [o2], [osq])
                    kb.op("dve", lambda e, gs=gs: e.tensor_reduce(
                        out=gs[:, 0:4], in_=osq[:].rearrange("p (h e) -> p h e", h=4), axis=AX.X, op=ALU.add), [osq], [gs])
                    kb.op("dve", lambda e, gs=gs: e.tensor_scalar(out=gs[:, 0:4], in0=gs[:, 0:4], scalar1=1.0 / 128,
                                                                  scalar2=EPS, op0=ALU.mult, op1=ALU.add), [gs], [gs])
                    kb.op("act", lambda e, gs=gs: e.activation(out=gs[:, 0:4], in_=gs[:, 0:4], func=AF.Sqrt), [gs], [gs])
                    kb.op("dve", lambda e, gs=gs: e.reciprocal(out=gs[:, 4:8], in_=gs[:, 0:4]), [gs], [gs])
                    kb.op("dve", lambda e, o2=o2, gs=gs: e.tensor_tensor(
                        out=o2[:].rearrange("p (h e) -> p h e", h=4), in0=o2[:].rearrange("p (h e) -> p h e", h=4),
                        in1=bass.AP(gs[:].tensor, gs[:].offset + 4, [list(gs[:].ap[0]), [1, 4], [0, 128]]), op=ALU.mult),
                        [o2, gs], [o2])
                    kb.op("pool", lambda e, o2=o2: e.tensor_tensor(
                        out=o2[:].rearrange("p (h e) -> p h e", h=4), in0=o2[:].rearrange("p (h e) -> p h e", h=4),
                        in1=bass.AP(gnb[:].tensor, gnb[:].offset, [list(gnb[:].ap[0]), [0, 4], [1, 128]]), op=ALU.mult),
                        [o2, gnb], [o2])
                    kb.op("act", lambda e, rt=rt: e.activation(out=rt[:], in_=rt[:], func=AF.Silu), [rt], [rt])
                    yb = ybb[fi]
                    kb.op("dve", lambda e, yb=yb, o2=o2, rt=rt: e.tensor_tensor(out=yb[:], in0=o2[:], in1=rt[:], op=ALU.mult),
                          [o2, rt], [yb])
                    pY = psum()
                    pY16 = ps16(pY)
                    for head in range(4):
                        kb.op("pe", lambda e, yb=yb, head=head, pY16=pY16: e.transpose(
                            out=pY16[:, head * 128:(head + 1) * 128], in_=yb[:, head * 128:(head + 1) * 128],
                            identity=identb[:]), [yb, identb], [pY], inc=(head == 3))
                    yT = ybT[fi]
                    kb.op("act", lambda e, yT=yT, pY16=pY16: e.copy(out=yT[:].rearrange("p h t -> p (h t)"), in_=pY16[:, 0:512]),
                          [pY], [yT])
                    kb.dma("pool", mixT_d[s][512:1024, cs].rearrange("(h p) t -> p h t", p=128), yT[:], [yT], [mixT_d[s]])
    kb.pop()
    if STOP == "M0":
        return finish()

    xp_d = [kb.dram("xp_%d" % s, [T, D], F32, okind) for s in range(NS)]

    def bc_rows(b_, n_mid, n_in):
        a_ = b_[:]
        return bass.AP(a_.tensor, a_.offset, [list(a_.ap[0]), [0, n_mid], [1, n_in]])

    def post_phase(l):
        kb.push()
        wpb = kb.sb([128, KC, D], BF16, "wpb")
        wsrc = wout_d if l == 0 else wo_d
        load_w_bf16(wpb, lambda c0, n: wpb[:, :, c0:c0 + n], wsrc, lambda c0, n: wsrc[:, :, c0:c0 + n], D)
        wr = kb.sb([128, KC, 20], F32, "wr")
        kb.dma("sp", wr[:], wr_d[l], [wr_d], [wr])
        brb = kb.sb([128, 20], F32, "brb")
        kb.dma("sp", brb[:], bass.AP(br_d[:].tensor, l * 20, [[0, 128], [1, 20]]), [br_d], [brb])
        H2s = [kb.sb([128, KC, 1024], BF16, "H2") for _ in range(2)]
        ACC = kb.sb([128, 8, D], F32, "ACC")
        COMBs = [kb.sb([128, 8, 16], F32, "COMB") for _ in range(2)]
        wring = [kb.sb([128, 4096], BF16, "wring") for _ in range(4)]
        HE = [kb.sb([128, 4, 512], BF16, "HE") for _ in range(2)]
        SIL = [kb.sb([128, 512], F32, "SIL") for _ in range(2)]
        Mb = [kb.sb([128, KC, 128], BF16, "Mb") for _ in range(3)]
        xtb = [kb.sb([128, D], F32, "xtb") for _ in range(3)]
        gtb = [kb.sb([128, D], F32, "gtb") for _ in range(3)]
        tmpbs = [kb.sb([128, D], F32, "tmpb") for _ in range(2)]
        xpb = [kb.sb([128, D], F32, "xpb") for _ in range(3)]
        h32s = [kb.sb([128, KC, 128], F32, "h32") for _ in range(2)]
        rt = [kb.sb([128, 64], F32, "rt") for _ in range(3)]
        tiles = []
        for s in range(NS):
            for t0 in range(0 if l == 0 else TC, T, 128):
                tiles.append((s, t0))
        blocks = [tiles[i:i + 8] for i in range(0, len(tiles), 8)]
        wc = {"w": 0, "k": 0, "x": 0}
        def pre_tile(i, s, t0, H2, COMB):
            for _once in (0,):
                v = NS if t0 < TC else s
                Mt = Mb[wc["x"] % 3]
                xt = xtb[wc["x"] % 3]
                g1 = gtb[wc["x"] % 3]
                xp = xpb[wc["x"] % 3]
                r_ = rt[wc["x"] % 3]
                tmpb = tmpbs[wc["x"] % 2]
                h32 = h32s[wc["x"] % 2]
                wc["x"] += 1
                if l == 0:
                    kb.dma("sp", Mt[:], mixT_d[s][:, t0:t0 + 128].rearrange("(k p) t -> p k t", p=128), [mixT_d[s]], [Mt])
                    sb_, sap = tile_src(s, t0)
                    kb.dma("sp", xt[:], sap, [sb_], [xt])
                else:
                    kb.dma("sp", Mt[:], attT_d[s][:, t0 - TC:t0 - TC + 128].rearrange("(k p) t -> p k t", p=128),
                           [attT_d[s]], [Mt])
                    kb.dma("sp", xt[:], x1_d[s][t0:t0 + 128, :], [x1_d[s]], [xt])
                kb.dma("sp", g1[:], gdram[(l, 0, v)][:], [gdram[(l, 0, v)]], [g1])
                for half in range(2):
                    pb = psum()
                    hs_ = slice(half * 512, (half + 1) * 512)
                    for kc in range(KC):
                        kb.op("pe", lambda e, pb=pb, kc=kc, Mt=Mt, hs_=hs_: e.matmul(
                            pb[:, :], lhsT=Mt[:, kc, :], rhs=wpb[:, kc, hs_], start=(kc == 0), stop=(kc == KC - 1)),
                            [Mt, wpb], [pb], inc=(kc == KC - 1))
                    kb.op("dve", lambda e, pb=pb, g1=g1, hs_=hs_: e.tensor_tensor(
                        out=tmpb[:, hs_], in0=pb[:, :], in1=g1[:, hs_], op=ALU.mult), [pb, g1], [tmpb])
                kb.op("dve", lambda e, xp=xp, xt=xt, tmpb=tmpb: e.tensor_tensor(out=xp[:], in0=tmpb[:], in1=xt[:], op=ALU.add),
                      [tmpb, xt], [xp])
                kb.dma("pool", xp_d[s][t0:t0 + 128, :], xp[:], [xp], [xp_d[s]])
                yield
                norm_mod_T(xp, xp[:], l, 1, v, H2, lambda j, i=i: H2[:, j, i * 128:(i + 1) * 128], h32=h32)
                yield
                pb = psum()
                for kc in range(KC):
                    kb.op("pe", lambda e, pb=pb, kc=kc: e.matmul(pb[:, 0:20], lhsT=h32[:, kc, :], rhs=wr[:, kc, :],
                                                                  start=(kc == 0), stop=(kc == KC - 1)),
                          [h32, wr], [pb], inc=(kc == KC - 1))
                dv = lambda fn, r_=r_: kb.op("dve", fn, [r_], [r_])
                kb.op("dve", lambda e, pb=pb, r_=r_: e.tensor_tensor(out=r_[:, 0:20], in0=pb[:, 0:20], in1=brb[:], op=ALU.add),
                      [pb, brb], [r_])
                dv(lambda e, r_=r_: e.tensor_reduce(out=r_[:, 20:21], in_=r_[:, 0:4], axis=AX.X, op=ALU.max))
                dv(lambda e, r_=r_: e.tensor_scalar(out=r_[:, 21:22], in0=r_[:, 20:21], scalar1=-1.0, scalar2=None, op0=ALU.mult))
                kb.op("act", lambda e, r_=r_: e.activation(out=r_[:, 24:28], in_=r_[:, 0:4], func=AF.Exp,
                                                           bias=r_[:, 21:22], accum_out=r_[:, 22:23]), [r_], [r_])
                dv(lambda e, r_=r_: e.reciprocal(out=r_[:, 23:24], in_=r_[:, 22:23]))
                dv(lambda e, r_=r_: e.tensor_scalar(out=r_[:, 24:28], in0=r_[:, 0:4], scalar1=r_[:, 20:21], scalar2=None,
                                                    op0=ALU.is_equal))
                dv(lambda e, r_=r_: e.tensor_scalar(out=r_[:, 24:28], in0=r_[:, 24:28], scalar1=BIG, scalar2=-BIG,
                                                    op0=ALU.mult, op1=ALU.add))
                dv(lambda e, r_=r_: e.tensor_tensor(
                    out=r_[:, 28:44].rearrange("p (g e) -> p g e", g=4), in0=r_[:, 4:20].rearrange("p (g e) -> p g e", g=4),
                    in1=bass.AP(r_[:].tensor, r_[:].offset + 24, [list(r_[:].ap[0]), [1, 4], [0, 4]]), op=ALU.add))
                dv(lambda e, r_=r_: e.max(out=r_[:, 44:52], in_=r_[:, 28:44]))
                dv(lambda e, r_=r_: e.tensor_tensor(out=r_[:, 52:53], in0=r_[:, 44:45], in1=r_[:, 45:46], op=ALU.subtract))
                kb.op("act", lambda e, r_=r_: e.activation(out=r_[:, 53:54], in_=r_[:, 52:53], func=AF.Sigmoid), [r_], [r_])
                kb.op("act", lambda e, r_=r_: e.activation(out=r_[:, 54:55], in_=r_[:, 52:53], func=AF.Sigmoid, scale=-1.0),
                      [r_], [r_])
                dv(lambda e, r_=r_: e.tensor_scalar(out=r_[:, 0:16], in0=r_[:, 28:44], scalar1=r_[:, 44:45], scalar2=r_[:, 53:54],
                                                    op0=ALU.is_equal, op1=ALU.mult))
                dv(lambda e, r_=r_: e.tensor_scalar(out=r_[:, 28:44], in0=r_[:, 28:44], scalar1=r_[:, 45:46], scalar2=r_[:, 54:55],
                                                    op0=ALU.is_equal, op1=ALU.mult))
                dv(lambda e, r_=r_: e.tensor_tensor(out=r_[:, 0:16], in0=r_[:, 0:16], in1=r_[:, 28:44], op=ALU.add))
                kb.op("dve", lambda e, r_=r_, i=i: e.tensor_scalar(out=COMB[:, i, :], in0=r_[:, 0:16], scalar1=r_[:, 23:24],
                                                                   scalar2=None, op0=ALU.mult), [r_], [COMB])
                yield

        def drain(g):
            for _ in g:
                pass

        run_pipeline((pre_tile(i, s, t0, H2s[0], COMBs[0]) for i, (s, t0) in enumerate(blocks[0])), 3)
        for bi, blk in enumerate(blocks):
            nb = len(blk)
            H2 = H2s[bi % 2]
            COMB = COMBs[bi % 2]
            bg = None
            bgl = []
            if bi + 1 < len(blocks):
                kb.rec = bgl
                pstate["set"] = [6, 7]
                run_pipeline((pre_tile(i, s, t0, H2s[(bi + 1) % 2], COMBs[(bi + 1) % 2])
                              for i, (s, t0) in enumerate(blocks[bi + 1])), 3)
                kb.rec = None
            pstate["set"] = [0, 1, 2, 3, 4, 5]
            nslots = NEXP * ((nb + 3) // 4) * 8
            per_slot = (len(bgl) + nslots - 1) // nslots + 1
            for ex in range(NEXP):
                wb = []
                for wi, wd in enumerate((w1_d, w3_d, w2_d)):
                    slot = wring[wc["w"] % 4]
                    wc["w"] += 1
                    wb.append(slot)
                    srcv = wd[l, ex].rearrange("p a b -> p (a b)")
                    for hh in range(2):
                        kb.dma("pool", slot[:, hh * 2048:(hh + 1) * 2048], srcv[:, hh * 2048:(hh + 1) * 2048],
                               [wd], [slot])
                W1 = wb[0][:].rearrange("p (k n) -> p k n", k=KC)
                W3 = wb[1][:].rearrange("p (k n) -> p k n", k=KC)
                W2 = wb[2][:].rearrange("p (k n) -> p k n", k=4)
                for g0 in range(0, nb, 4):
                    ntile = min(4, nb - g0)
                    n = ntile * 128
                    c0 = g0 * 128
                    he = HE[wc["k"] % 2]
                    wc["k"] += 1
                    for hc in range(4):
                        pb1 = psum()
                        pb3 = psum()
                        for (pbx, Wx, wbuf) in ((pb1, W1, wb[0]), (pb3, W3, wb[1])):
                            for kc in range(KC):
                                kb.op("pe", lambda e, pbx=pbx, Wx=Wx, kc=kc, hc=hc, c0=c0, n=n: e.matmul(
                                    pbx[:, 0:n], lhsT=Wx[:, kc, hc * 128:(hc + 1) * 128], rhs=H2[:, kc, c0:c0 + n],
                                    start=(kc == 0), stop=(kc == KC - 1)), [wbuf, H2], [pbx], inc=(kc == KC - 1))
                        sil = SIL[hc % 2]
                        kb.op("act", lambda e, sil=sil, pb1=pb1, n=n: e.activation(out=sil[:, 0:n], in_=pb1[:, 0:n], func=AF.Silu),
                              [pb1], [sil])
                        kb.op("dve", lambda e, he=he, hc=hc, sil=sil, pb3=pb3, n=n: e.tensor_tensor(
                            out=he[:, hc, 0:n], in0=sil[:, 0:n], in1=pb3[:, 0:n], op=ALU.mult), [sil, pb3], [he])
                        kb.replay(bgl, per_slot)
                    for ii in range(ntile):
                        i = g0 + ii
                        for half in range(2):
                            hs_ = slice(half * 512, (half + 1) * 512)
                            pbo = psum()
                            for hc in range(4):
                                kb.op("pe", lambda e, pbo=pbo, he=he, hc=hc, ii=ii, W2=W2, hs_=hs_: e.matmul(
                                    pbo[:, :], lhsT=he[:, hc, ii * 128:(ii + 1) * 128], rhs=W2[:, hc, hs_],
                                    start=(hc == 0), stop=(hc == 3)), [he, wb[2]], [pbo], inc=(hc == 3))
                            if ex == 0:
                                kb.op("dve", lambda e, pbo=pbo, i=i, hs_=hs_, ex=ex: e.tensor_scalar(
                                    out=ACC[:, i, hs_], in0=pbo[:, :], scalar1=COMB[:, i, ex:ex + 1], scalar2=None,
                                    op0=ALU.mult), [pbo, COMB], [ACC])
                            else:
                                kb.op("dve", lambda e, pbo=pbo, i=i, hs_=hs_, ex=ex: e.scalar_tensor_tensor(
                                    out=ACC[:, i, hs_], in0=pbo[:, :], scalar=COMB[:, i, ex:ex + 1], in1=ACC[:, i, hs_],
                                    op0=ALU.mult, op1=ALU.add), [pbo, COMB, ACC], [ACC])
                        kb.replay(bgl, per_slot)
            kb.replay(bgl, len(bgl) + 1)
            pstate["set"] = list(range(8))
            def fin_tile(i, s, t0):
                v = NS if t0 < TC else s
                xt = xtb[wc["x"] % 3]
                g2 = gtb[wc["x"] % 3]
                xo = xpb[wc["x"] % 3]
                wc["x"] += 1
                kb.dma("sp", xt[:], xp_d[s][t0:t0 + 128, :], [xp_d[s]], [xt])
                kb.dma("sp", g2[:], gdram[(l, 1, v)][:], [gdram[(l, 1, v)]], [g2])
                yield
                kb.op("dve", lambda e, g2=g2, i=i: e.tensor_tensor(out=g2[:], in0=ACC[:, i, :], in1=g2[:], op=ALU.mult),
                      [ACC, g2], [g2])
                kb.op("dve", lambda e, xo=xo, g2=g2, xt=xt: e.tensor_tensor(out=xo[:], in0=g2[:], in1=xt[:], op=ALU.add),
                      [g2, xt], [xo])
                if l == 0:
                    kb.dma("pool", x1_d[s][t0:t0 + 128, :], xo[:], [xo], [x1_d[s]])
                else:
                    kb.dma("pool", out_d[s, t0 - TC:t0 - TC + 128, :], xo[:], [xo], [out_d])

            run_pipeline((fin_tile(i, s, t0) for i, (s, t0) in enumerate(blk)), 3)
        kb.pop()

    post_phase(0)
    if STOP == "P1":
        return finish()

    kb.push()
    wqb = kb.sb([128, KC, 1536], BF16, "wqb")
    load_w_bf16(wqb, lambda c0, n: wqb[:, :, c0:c0 + n], wqkv_d, lambda c0, n: wqkv_d[:, :, c0:c0 + n], 1536)
    qrow = kb.sb([128, 128], F32, "qrow")
    krow = kb.sb([128, 128], F32, "krow")
    kb.dma("sp", qrow[:], bass.AP(qn_d[:].tensor, 0, [[0, 128], [1, 128]]), [qn_d], [qrow])
    kb.dma("sp", krow[:], bass.AP(kn_d[:].tensor, 0, [[0, 128], [1, 128]]), [kn_d], [krow])
    kb.op("dve", lambda e: e.tensor_scalar(out=qrow[:], in0=qrow[:], scalar1=128.0 ** -0.5, scalar2=None, op0=ALU.mult),
          [qrow], [qrow])
    xt1 = [kb.sb([128, D], F32, "xt1") for _ in range(3)]
    hT1 = [kb.sb([128, KC, 128], BF16, "hT1") for _ in range(3)]
    sqbs = [kb.sb([128, 512], F32, "sqb") for _ in range(2)]
    ssb = [kb.sb([128, 32], F32, "ssb") for _ in range(3)]
    qnb = [kb.sb([128, D], F32, "qnb") for _ in range(3)]
    knb = [kb.sb([128, 256], F32, "knb") for _ in range(3)]
    cosb = [kb.sb([128, 64], F32, "cosb") for _ in range(3)]
    sinb = [kb.sb([128, 64], F32, "sinb") for _ in range(3)]
    rAs = [kb.sb([128, 512], F32, "rA") for _ in range(2)]
    rBs = [kb.sb([128, 512], F32, "rB") for _ in range(2)]
    rCs = [kb.sb([128, 512], F32, "rC") for _ in range(2)]
    rDs = [kb.sb([128, 512], F32, "rD") for _ in range(2)]
    qrb = [kb.sb([128, D], BF16, "qrb") for _ in range(3)]
    krb = [kb.sb([128, 256], BF16, "krb") for _ in range(3)]
    vxb = [kb.sb([128, 2, 130], BF16, "vxb") for _ in range(3)]
    qTt = [kb.sb([128, 8, 128], BF16, "qTt") for _ in range(3)]
    kTt = [kb.sb([128, 2, 128], BF16, "kTt") for _ in range(3)]
    for bb in vxb:
        kb.op("pool", lambda e, bb=bb: e.memset(bb[:], 1.0), [], [bb])

    def sview(b_, nh, off):
        a_ = b_[:]
        return bass.AP(a_.tensor, a_.offset + off, [list(a_.ap[0]), [128, nh], [2, 64]])

    def cview(b_, nh):
        a_ = b_[:]
        return bass.AP(a_.tensor, a_.offset, [list(a_.ap[0]), [0, nh], [1, 64]])

    pcs = {"i": 0}

    def p1b_task(s, t0):
            is_ctx = t0 < TC
            v = NS if is_ctx else s
            k_ = pcs["i"] % 3
            pcs["i"] += 1
            sqb = sqbs[k_ % 2]
            rA, rB, rC, rD = rAs[k_ % 2], rBs[k_ % 2], rCs[k_ % 2], rDs[k_ % 2]
            xt = xt1[k_]
            hT = hT1[k_]
            ss = ssb[k_]
            kb.dma("sp", xt[:], x1_d[s][t0:t0 + 128, :], [x1_d[s]], [xt])
            norm_mod_T(xt, xt[:], 1, 0, v, hT, lambda j, hT=hT: hT[:, j, :])
            banks = []
            for b in ((2,) if is_ctx else (0, 1, 2)):
                pb = psum()
                banks.append((b, pb))
                for kc in range(KC):
                    kb.op("pe", lambda e, pb=pb, kc=kc, b=b, hT=hT: e.matmul(
                        pb[:, :], lhsT=hT[:, kc, :], rhs=wqb[:, kc, b * 512:(b + 1) * 512],
                        start=(kc == 0), stop=(kc == KC - 1)), [hT, wqb], [pb], inc=(kc == KC - 1))
            yield
            for (b, pb) in banks:
                if b < 2:
                    kb.op("act", lambda e, pb=pb: e.activation(out=sqb[:], in_=pb[:, :], func=AF.Square), [pb], [sqb])
                    kb.op("dve", lambda e, ss=ss, b=b: e.tensor_reduce(
                        out=ss[:, b * 4:(b + 1) * 4], in_=sqb[:].rearrange("p (h e) -> p h e", h=4), axis=AX.X, op=ALU.add),
                        [sqb], [ss])
                else:
                    kb.op("act", lambda e, pb=pb: e.activation(out=sqb[:, 0:256], in_=pb[:, 0:256], func=AF.Square), [pb], [sqb])
                    kb.op("dve", lambda e, ss=ss: e.tensor_reduce(
                        out=ss[:, 8:10], in_=sqb[:, 0:256].rearrange("p (h e) -> p h e", h=2), axis=AX.X, op=ALU.add),
                        [sqb], [ss])
            kb.op("dve", lambda e, ss=ss: e.tensor_scalar(out=ss[:, 0:10], in0=ss[:, 0:10], scalar1=1.0 / 128, scalar2=EPS,
                                                          op0=ALU.mult, op1=ALU.add), [ss], [ss])
            kb.op("act", lambda e, ss=ss: e.activation(out=ss[:, 0:10], in_=ss[:, 0:10], func=AF.Sqrt), [ss], [ss])
            kb.op("dve", lambda e, ss=ss: e.reciprocal(out=ss[:, 16:26], in_=ss[:, 0:10]), [ss], [ss])
            qn = qnb[k_]
            kn = knb[k_]
            vx = vxb[k_]
            for (b, pb) in banks:
                if b < 2:
                    kb.op("dve", lambda e, pb=pb, b=b, qn=qn, ss=ss: e.tensor_tensor(
                        out=qn[:, b * 512:(b + 1) * 512].rearrange("p (h e) -> p h e", h=4),
                        in0=pb[:, :].rearrange("p (h e) -> p h e", h=4),
                        in1=bass.AP(ss[:].tensor, ss[:].offset + 16 + b * 4, [list(ss[:].ap[0]), [1, 4], [0, 128]]),
                        op=ALU.mult), [pb, ss], [qn])
                else:
                    kb.op("dve", lambda e, pb=pb, kn=kn, ss=ss: e.tensor_tensor(
                        out=kn[:].rearrange("p (h e) -> p h e", h=2),
                        in0=pb[:, 0:256].rearrange("p (h e) -> p h e", h=2),
                        in1=bass.AP(ss[:].tensor, ss[:].offset + 24, [list(ss[:].ap[0]), [1, 2], [0, 128]]),
                        op=ALU.mult), [pb, ss], [kn])
                    kb.op("act", lambda e, pb=pb, vx=vx: e.copy(out=vx[:, :, 0:128],
                                                                in_=pb[:, 256:512].rearrange("p (h e) -> p h e", h=2)),
                          [pb], [vx])
            kb.op("pool", lambda e, kn=kn: e.tensor_tensor(out=kn[:].rearrange("p (h e) -> p h e", h=2),
                                                           in0=kn[:].rearrange("p (h e) -> p h e", h=2),
                                                           in1=bc_rows(krow, 2, 128), op=ALU.mult), [kn, krow], [kn])
            yield
            kr = krb[k_]
            if is_ctx:
                kb.op("act", lambda e, kr=kr, kn=kn: e.copy(out=kr[:], in_=kn[:]), [kn], [kr])
            else:
                qr = qrb[k_]
                cs_ = cosb[k_]
                sn_ = sinb[k_]
                tl0 = t0 - TC
                kb.dma("sp", cs_[:], cos_d[tl0:tl0 + 128, :], [cos_d], [cs_])
                kb.dma("sp", sn_[:], sin_d[tl0:tl0 + 128, :], [sin_d], [sn_])
                kb.op("pool", lambda e, qn=qn: e.tensor_tensor(out=qn[:].rearrange("p (h e) -> p h e", h=8),
                                                               in0=qn[:].rearrange("p (h e) -> p h e", h=8),
                                                               in1=bc_rows(qrow, 8, 128), op=ALU.mult), [qn, qrow], [qn])
                for (src_, dst_, nh) in ((qn, qr, 8), (kn, kr, 2)):
                    nn = nh * 64
                    vA = rA[:, 0:nn].rearrange("p (h i) -> p h i", h=nh)
                    vB = rB[:, 0:nn].rearrange("p (h i) -> p h i", h=nh)
                    vC = rC[:, 0:nn].rearrange("p (h i) -> p h i", h=nh)
                    vD = rD[:, 0:nn].rearrange("p (h i) -> p h i", h=nh)
                    x1v, x2v = sview(src_, nh, 0), sview(src_, nh, 1)
                    cv_, sv_ = cview(cs_, nh), cview(sn_, nh)
                    kb.op("dve", lambda e, vA=vA, x1v=x1v, cv_=cv_: e.tensor_tensor(out=vA, in0=x1v, in1=cv_, op=ALU.mult),
                          [src_, cs_], [rA])
                    kb.op("pool", lambda e, vB=vB, x2v=x2v, sv_=sv_: e.tensor_tensor(out=vB, in0=x2v, in1=sv_, op=ALU.mult),
                          [src_, sn_], [rB])
                    kb.op("pool", lambda e, vC=vC, x1v=x1v, sv_=sv_: e.tensor_tensor(out=vC, in0=x1v, in1=sv_, op=ALU.mult),
                          [src_, sn_], [rC])
                    kb.op("dve", lambda e, vD=vD, x2v=x2v, cv_=cv_: e.tensor_tensor(out=vD, in0=x2v, in1=cv_, op=ALU.mult),
                          [src_, cs_], [rD])
                    kb.op("dve", lambda e, dst_=dst_, nh=nh, vA=vA, vB=vB: e.tensor_tensor(
                        out=sview(dst_, nh, 0), in0=vA, in1=vB, op=ALU.subtract), [rA, rB], [dst_])
                    kb.op("pool", lambda e, dst_=dst_, nh=nh, vC=vC, vD=vD: e.tensor_tensor(
                        out=sview(dst_, nh, 1), in0=vC, in1=vD, op=ALU.add), [rC, rD], [dst_])
                yield
                pq = psum()
                pq16 = ps16(pq)
                for h in range(8):
                    kb.op("pe", lambda e, pq16=pq16, qr=qr, h=h: e.transpose(
                        out=pq16[:, h * 128:(h + 1) * 128], in_=qr[:, h * 128:(h + 1) * 128], identity=identb[:]),
                        [qr, identb], [pq], inc=(h == 7))
                qT_ = qTt[k_]
                kb.op("act", lambda e, qT_=qT_, pq16=pq16: e.copy(out=qT_[:].rearrange("p h t -> p (h t)"), in_=pq16[:, :]),
                      [pq], [qT_])
                kb.dma("pool", q1T_d[s][:, :, tl0:tl0 + 128], qT_[:], [qT_], [q1T_d[s]])
            pk = psum()
            pk16 = ps16(pk)
            for h in range(2):
                kb.op("pe", lambda e, pk16=pk16, kr=kr, h=h: e.transpose(
                    out=pk16[:, h * 128:(h + 1) * 128], in_=kr[:, h * 128:(h + 1) * 128], identity=identb[:]),
                    [kr, identb], [pk], inc=(h == 1))
            kT_ = kTt[k_]
            kb.op("dve", lambda e, kT_=kT_, pk16=pk16: e.tensor_copy(out=kT_[:].rearrange("p h t -> p (h t)"), in_=pk16[:, 0:256]),
                  [pk], [kT_])
            kb.dma("pool", k1T_d[s][:, :, t0:t0 + 128], kT_[:], [kT_], [k1T_d[s]])
            kb.dma("pool", v1_d[s][t0:t0 + 128, :, :], vx[:], [vx], [v1_d[s]])

    run_pipeline((p1b_task(s, t0) for s in range(NS) for t0 in range(0, T, 128)), 3)
    kb.pop()
    if STOP == "P1b":
        return finish()

    kb.push()
    K1 = kb.sb([128, 2, T], BF16, "K1")
    V1 = kb.sb([128, NT, 2, 130], BF16, "V1")
    Qb = [kb.sb([128, 8, 512], BF16, "Qb") for _ in range(2)]
    pTb = [kb.sb([128, 512], BF16, "pTb") for _ in range(3)]
    atok = [kb.sb([128, D], BF16, "atok") for _ in range(4)]
    rsb = [kb.sb([128, 4], F32, "rsb") for _ in range(2)]
    aTt = [kb.sb([128, 8, 128], BF16, "aTt") for _ in range(2)]
    mc = {"q": 0, "p": 0, "s": 0, "r": 0, "a": 0}
    for s in range(NS):
        kb.dma("sp", K1[:], k1T_d[s][:], [k1T_d[s]], [K1])
        kb.dma("sp", V1[:], v1_d[s][:].rearrange("(n p) h e -> p n h e", p=128), [v1_d[s]], [V1])
        for q0 in range(0, TL, 512):
            nq = min(512, TL - q0)
            nqt = nq // 128
            Q = Qb[mc["q"] % 2]
            mc["q"] += 1
            kb.dma("sp", Q[:, :, 0:nq], q1T_d[s][:, :, q0:q0 + nq], [q1T_d[s]], [Q])
            its = [(h, kc) for h in range(8) for kc in range(NT)]

            def qk(idx, Q=Q, nq=nq, its=its):
                h, kc = its[idx]
                kv = h // 4
                sT = PSB[4 + idx % 4]
                kb.op("pe", lambda e, sT=sT, kv=kv, kc=kc, h=h: e.matmul(
                    sT[:, 0:nq], lhsT=K1[:, kv, kc * 128:(kc + 1) * 128], rhs=Q[:, h, 0:nq], start=True, stop=True),
                    [K1, Q], [sT])

            LA = 2
            for idx in range(min(LA, len(its))):
                qk(idx)
            for idx, (h, kc) in enumerate(its):
                kv = h // 4
                if idx + LA < len(its):
                    qk(idx + LA)
                sT = PSB[4 + idx % 4]
                pT = pTb[idx % 3]
                kb.op("act", lambda e, pT=pT, sT=sT, nq=nq: e.activation(out=pT[:, 0:nq], in_=sT[:, 0:nq], func=AF.Exp),
                      [sT], [pT])
                for qt in range(nqt):
                    kb.op("pe", lambda e, qt=qt, pT=pT, kc=kc, kv=kv: e.matmul(
                        PSB[qt][:, 0:129], lhsT=pT[:, qt * 128:(qt + 1) * 128], rhs=V1[:, kc, kv, 0:129],
                        start=(kc == 0), stop=(kc == NT - 1)), [pT, V1], [PSB[qt]], inc=(qt == nqt - 1))
                if kc == NT - 1:
                    rs = rsb[mc["r"] % 2]
                    mc["r"] += 1
                    for qt in range(nqt):
                        kb.op("dve", lambda e, rs=rs, qt=qt: e.reciprocal(out=rs[:, qt:qt + 1], in_=PSB[qt][:, 128:129]),
                              [PSB[qt]], [rs])
                        kb.op("dve", lambda e, rs=rs, qt=qt, h=h: e.tensor_scalar(
                            out=atok[qt][:, h * 128:(h + 1) * 128], in0=PSB[qt][:, 0:128], scalar1=rs[:, qt:qt + 1],
                            scalar2=None, op0=ALU.mult), [PSB[qt], rs], [atok[qt]])
            for qt in range(nqt):
                pa = PSB[4 + mc["s"] % 4]
                mc["s"] += 1
                pa16 = ps16(pa)
                for h in range(8):
                    kb.op("pe", lambda e, pa16=pa16, qt=qt, h=h: e.transpose(
                        out=pa16[:, h * 128:(h + 1) * 128], in_=atok[qt][:, h * 128:(h + 1) * 128], identity=identb[:]),
                        [atok[qt], identb], [pa], inc=(h == 7))
                aT = aTt[mc["a"] % 2]
                mc["a"] += 1
                kb.op("dve", lambda e, aT=aT, pa16=pa16: e.tensor_copy(out=aT[:].rearrange("p h t -> p (h t)"), in_=pa16[:, :]),
                      [pa], [aT])
                tq = q0 + qt * 128
                kb.dma("pool", attT_d[s][:, tq:tq + 128].rearrange("(k p) t -> p k t", p=128), aT[:], [aT], [attT_d[s]])
    kb.pop()
    if STOP == "M1":
        return finish()

    post_phase(1)
    return finish()


def host_layout(inputs, core, NS, TC, TL):
    f = lambda a: np.ascontiguousarray(np.asarray(a, dtype=np.float32))
    b0 = core * NS
    m = {}
    m["x"] = f(inputs["x"][b0:b0 + NS, :TL])
    m["ctx"] = f(inputs["ctx"][b0:b0 + NS, :TC])
    cvs = np.concatenate([np.asarray(inputs["c"])[b0:b0 + NS], np.asarray(inputs["c_ctx"])[None]], 0)
    m["cvec"] = f(cvs.reshape(NS + 1, KC, 128).transpose(2, 1, 0))
    m["norm1c"] = f(np.asarray(inputs["norm1"]).reshape(2, KC, 128).transpose(0, 2, 1))
    m["norm2c"] = f(np.asarray(inputs["norm2"]).reshape(2, KC, 128).transpose(0, 2, 1))
    m["w_ada"] = f(np.asarray(inputs["w_ada"]).reshape(2, KC, 128, 6 * D).transpose(0, 2, 1, 3))
    m["b_adac"] = f(np.asarray(inputs["b_ada"]).reshape(2, 48, 128).transpose(0, 2, 1))
    m["w_in"] = f(np.asarray(inputs["ev_w_in"])[0].reshape(KC, 128, 2592).transpose(1, 0, 2))
    m["conv_w"] = f(np.asarray(inputs["ev_conv_w"])[0].reshape(4, 4, 128).transpose(2, 1, 0))
    m["conv_b"] = f(np.asarray(inputs["ev_conv_b"])[0].reshape(4, 128).T)
    for nm_, key in (("lru_wa", "ev_lru_wa"), ("lru_wi", "ev_lru_wi")):
        w = np.asarray(inputs[key])[0]
        bd = np.zeros((2, 4, 128, 128), np.float32)
        for d in range(2):
            for blk in range(8):
                cc, h = blk // 2, blk % 2
                bd[d, cc, h * 64:(h + 1) * 64, h * 64:(h + 1) * 64] = w[d, blk]
        m[nm_] = bd
    for nm_, key in (("lru_ba", "ev_lru_ba"), ("lru_bi", "ev_lru_bi"), ("lru_lam", "ev_lru_lam")):
        m[nm_] = f(np.asarray(inputs[key])[0].reshape(2, 4, 128).transpose(2, 0, 1))
    m["gla_wg"] = f(np.asarray(inputs["ev_gla_wg"])[0])
    m["gla_bg"] = f(np.asarray(inputs["ev_gla_bg"])[0].reshape(2, 2, 128).transpose(2, 0, 1))
    m["gla_norm"] = f(np.asarray(inputs["ev_gla_norm"])[0][None])
    m["w_out"] = f(np.asarray(inputs["ev_w_out"])[0].reshape(KC, 128, D).transpose(1, 0, 2))
    m["w_qkv"] = f(np.asarray(inputs["od_w_qkv"])[0].reshape(KC, 128, 1536).transpose(1, 0, 2))
    m["q_norm"] = f(np.asarray(inputs["od_q_norm"])[0][None])
    m["k_norm"] = f(np.asarray(inputs["od_k_norm"])[0][None])
    m["w_o"] = f(np.asarray(inputs["od_w_o"])[0].reshape(KC, 128, D).transpose(1, 0, 2))
    wr = np.concatenate([np.asarray(inputs["moe_wg"]), np.asarray(inputs["moe_we"])], -1)
    m["moe_wr"] = f(wr.reshape(2, KC, 128, 20).transpose(0, 2, 1, 3))
    m["moe_br"] = f(np.concatenate([np.asarray(inputs["moe_bg"]), np.asarray(inputs["moe_be"])], -1)[:, None, :])
    m["moe_w1"] = f(np.asarray(inputs["moe_w1"]).reshape(2, NEXP, KC, 128, HID).transpose(0, 1, 3, 2, 4))
    m["moe_w3"] = f(np.asarray(inputs["moe_w3"]).reshape(2, NEXP, KC, 128, HID).transpose(0, 1, 3, 2, 4))
    m["moe_w2"] = f(np.asarray(inputs["moe_w2"]).reshape(2, NEXP, 4, 128, D).transpose(0, 1, 3, 2, 4))
    m["ident"] = np.eye(128, dtype=np.float32)
    jj, ii = np.meshgrid(np.arange(128), np.arange(128), indexing="ij")
    m["maskf"] = (jj <= ii).astype(np.float32)
    m["maskb"] = (jj >= ii).astype(np.float32)
    t = np.arange(TL)
    row = (t // 64).astype(np.float32)
    col = (t % 64).astype(np.float32)
    freqs = (10000.0 ** (-np.arange(32, dtype=np.float32) / 32)).astype(np.float32)
    ang = np.concatenate([row[:, None] * freqs, col[:, None] * freqs], -1).astype(np.float32)
    m["rope_cos"] = np.cos(ang).astype(np.float32)
    m["rope_sin"] = np.sin(ang).astype(np.float32)
    return m


_CACHE = {}


def kernel(**inputs):
    NS, TC, TL = 2, 256, 4096
    ncores = 8
    cfg = {"NS": NS, "TC": TC, "TL": TL}
    nc = build(cfg)
    in_maps = [host_layout(inputs, c, NS, TC, TL) for c in range(ncores)]
    res = run_bass_kernel_spmd(nc, in_maps, core_ids=list(range(ncores)))
    out = np.concatenate([r["out"] for r in res.results], axis=0)
    return out.astype(np.float32)
```

```python
import numpy as np
from contextlib import ExitStack
import concourse.bass as bass
import concourse.mybir as mybir
from concourse.bass_utils import run_bass_kernel_spmd
from concourse.alu_op_type import AluOpType as ALU

AF = mybir.ActivationFunctionType
AX = mybir.AxisListType
F32 = mybir.dt.float32
BF16 = mybir.dt.bfloat16
I32 = mybir.dt.int32

D = 1024
KC = 8
EPS = 1e-6
NEXP = 16
HID = 512
BIG = 1.0e30


class Buf:
    __slots__ = ("t", "w", "r", "name")

    def __init__(self, t, name=""):
        self.t = t
        self.w = None
        self.r = {}
        self.name = name

    def __getitem__(self, idx):
        return self.t[idx]


class KB:
    ND = 40

    def __init__(self, nc, es):
        self.nc = nc
        self.es = es
        self.E = {"pe": nc.tensor, "act": nc.scalar, "dve": nc.vector, "pool": nc.gpsimd, "sp": nc.sync}
        self.csem = {e: es.enter_context(nc.semaphore("c_" + e)) for e in ("pe", "act", "dve", "pool")}
        self.ccnt = {e: 0 for e in self.csem}
        self.dsem = [es.enter_context(nc.semaphore("d%d" % i)) for i in range(self.ND)]
        self.dval = [0] * self.ND
        self.di = 0
        self.seen = {e: {} for e in self.E}
        self.nbuf = 0
        self.ninst = 0
        self.phase = None
        import os as _os
        self.maxops = int(_os.environ.get("KB_MAXOPS", "1000000000"))
        self.skipped = False
        self.pend_inc = {}

    def sb(self, shape, dt, name=None):
        self.nbuf += 1
        name = (name or "b") + "_%d" % self.nbuf
        stack = self.phase if self.phase is not None else self.es
        return Buf(stack.enter_context(self.nc.sbuf_tensor(name, list(shape), dt)), name)

    def push(self):
        assert self.phase is None
        self.phase = ExitStack()

    def pop(self):
        self.barrier()
        self.phase.close()
        self.phase = None

    def barrier(self):
        for eng in self.E:
            for e2 in self.csem:
                v = self.ccnt[e2]
                if v > self.seen[eng].get(e2, 0):
                    self.E[eng].wait_ge(self.csem[e2], v)
                    self.seen[eng][e2] = v
            for i in range(self.ND):
                v = self.dval[i]
                if v > self.seen[eng].get(("d", i), 0):
                    self.E[eng].wait_ge(self.dsem[i], v)
                    self.seen[eng][("d", i)] = v

    def ps(self, shape, dt, name=None):
        self.nbuf += 1
        name = (name or "p") + "_%d" % self.nbuf
        return Buf(self.es.enter_context(self.nc.psum_tensor(name, list(shape), dt)), name)

    def dram(self, name, shape, dt, kind="Internal"):
        return Buf(self.nc.dram_tensor(name, list(shape), dt, kind=kind).ap(), name)

    def semobj(self, sk):
        return self.csem[sk] if isinstance(sk, str) else self.dsem[sk[1]]

    def _deps(self, eng, reads, writes):
        need = {}
        for b in reads:
            if b.w is not None:
                need[b.w[0]] = max(need.get(b.w[0], 0), b.w[1])
        for b in writes:
            if b.w is not None:
                need[b.w[0]] = max(need.get(b.w[0], 0), b.w[1])
            for sk, v in b.r.items():
                need[sk] = max(need.get(sk, 0), v)
        for sk, v in need.items():
            if eng == "pe" and sk == "pe":
                continue
            if self.seen[eng].get(sk, 0) >= v:
                continue
            self.E[eng].wait_ge(self.semobj(sk), v)
            self.seen[eng][sk] = v

    def _done(self, tok, reads, writes):
        for b in reads:
            b.r[tok[0]] = max(b.r.get(tok[0], 0), tok[1])
        for b in writes:
            b.w = tok
            b.r = {}

    def op(self, eng, fn, reads=(), writes=(), inc=True):
        if self.ninst >= self.maxops:
            if not self.skipped:
                self.skipped = True
                print("KB: first skipped op", eng, [b.name for b in reads], [b.name for b in writes])
            if inc and self.pend_inc.get(eng):
                pass
            return
        self._deps(eng, reads, writes)
        ins = fn(self.E[eng])
        self.ninst += 1
        if inc:
            self.ccnt[eng] += 1
            ins.then_inc(self.csem[eng], 1)
            tok = (eng, self.ccnt[eng])
        else:
            tok = (eng, self.ccnt[eng] + 1)
        self._done(tok, reads, writes)

    def dma(self, q, out, in_, reads=(), writes=(), **kw):
        if self.ninst >= self.maxops:
            if not self.skipped:
                self.skipped = True
                print("KB: first skipped dma", q, [b.name for b in reads], [b.name for b in writes])
            return
        i = self.di % self.ND
        self.di += 1
        self._deps(q, reads, writes)
        sk = ("d", i)
        if self.dval[i] > 0 and self.seen[q].get(sk, 0) < self.dval[i]:
            self.E[q].wait_ge(self.dsem[i], self.dval[i])
            self.seen[q][sk] = self.dval[i]
        self.E[q].dma_start(out=out, in_=in_, **kw).then_inc(self.dsem[i], 16)
        self.ninst += 1
        self.dval[i] += 16
        self._done((sk, self.dval[i]), reads, writes)

    def wait_all(self, eng, bufs):
        self._deps(eng, bufs, ())


def bcast(ap, dims):
    raise NotImplementedError


def mkap(t_ap, pat, off=0):
    return bass.AP(t_ap.tensor, t_ap.offset + off, [list(t_ap.ap[0])] + [list(p) for p in pat])


def run_pipeline(gens, depth):
    active = []
    it = iter(gens)
    done = False
    while True:
        if not done and len(active) < depth:
            try:
                active.append(next(it))
            except StopIteration:
                done = True
        if not active:
            if done:
                break
            continue
        for g in list(active):
            try:
                next(g)
            except StopIteration:
                active.remove(g)


def token_groups(TC, TL, g=512):
    out = []
    t = 0
    while t < TC:
        n = min(g, TC - t)
        out.append((t, n))
        t += n
    while t < TC + TL:
        n = min(g, TC + TL - t)
        out.append((t, n))
        t += n
    return out


def build(cfg):
    NS, TC, TL = cfg["NS"], cfg["TC"], cfg["TL"]
    DBG = cfg.get("DBG", False)
    STOP = cfg.get("STOP", "end")
    T = TC + TL
    NT = T // 128
    NTC = TC // 128
    NV = NS + 1
    nc = bass.Bass("TRN2", target_bir_lowering=False)
    es = ExitStack()
    kb = KB(nc, es)
    okind = "ExternalOutput" if DBG else "Internal"

    def din(name, shape, dt=F32):
        return Buf(nc.dram_tensor(name, list(shape), dt, kind="ExternalInput").ap(), name)

    x_d = din("x", [NS, TL, D])
    ctx_d = din("ctx", [NS, TC, D])
    cv_d = din("cvec", [128, KC, NV])
    n1_d = din("norm1c", [2, 128, KC])
    n2_d = din("norm2c", [2, 128, KC])
    wada_d = din("w_ada", [2, 128, KC, 6 * D])
    bada_d = din("b_adac", [2, 128, 48])
    win_d = din("w_in", [128, KC, 2592])
    convw_d = din("conv_w", [128, 4, 4])
    convb_d = din("conv_b", [128, 4])
    lwa_d = din("lru_wa", [2, 4, 128, 128])
    lwi_d = din("lru_wi", [2, 4, 128, 128])
    lba_d = din("lru_ba", [128, 2, 4])
    lbi_d = din("lru_bi", [128, 2, 4])
    llam_d = din("lru_lam", [128, 2, 4])
    gwg_d = din("gla_wg", [2, 16, 256])
    gbg_d = din("gla_bg", [128, 2, 2])
    gnorm_d = din("gla_norm", [1, 128])
    wout_d = din("w_out", [128, KC, D])
    wqkv_d = din("w_qkv", [128, KC, 1536])
    qn_d = din("q_norm", [1, 128])
    kn_d = din("k_norm", [1, 128])
    wo_d = din("w_o", [128, KC, D])
    wr_d = din("moe_wr", [2, 128, KC, 20])
    br_d = din("moe_br", [2, 1, 20])
    w1_d = din("moe_w1", [2, NEXP, 128, KC, HID])
    w3_d = din("moe_w3", [2, NEXP, 128, KC, HID])
    w2_d = din("moe_w2", [2, NEXP, 128, 4, D])
    ident_d = din("ident", [128, 128])
    maskf_d = din("maskf", [128, 128])
    maskb_d = din("maskb", [128, 128])
    cos_d = din("rope_cos", [TL, 64])
    sin_d = din("rope_sin", [TL, 64])
    out_d = Buf(nc.dram_tensor("out", [NS, TL, D], F32, kind="ExternalOutput").ap(), "out")

    xaT_d = [kb.dram("xaT%d" % s, [512, T], F32, okind) for s in range(NS)]
    gaT_d = [kb.dram("gaT%d" % s, [512, T], F32, okind) for s in range(NS)]
    qT_d = [kb.dram("qT%d" % s, [256, T], F32, okind) for s in range(NS)]
    kT_d = [kb.dram("kT%d" % s, [256, T], F32, okind) for s in range(NS)]
    lrT_d = [kb.dram("lrT%d" % s, [2, 16, T], F32, okind) for s in range(NS)]
    v_d = [kb.dram("vtok%d" % s, [T, 512], BF16, okind) for s in range(NS)]
    r_d = [kb.dram("rtok%d" % s, [T, 512], F32, okind) for s in range(NS)]
    of_d = [kb.dram("ofirst%d" % s, [T, 512], F32, okind) for s in range(NS)]
    mixT_d = [kb.dram("mixT%d" % s, [D, T], BF16, okind) for s in range(NS)]
    x1_d = [kb.dram("x1_%d" % s, [T, D], F32, okind) for s in range(NS)]
    q1T_d = [kb.dram("q1T%d" % s, [128, 8, TL], BF16, okind) for s in range(NS)]
    k1T_d = [kb.dram("k1T%d" % s, [128, 2, T], BF16, okind) for s in range(NS)]
    v1_d = [kb.dram("v1e%d" % s, [T, 2, 130], BF16, okind) for s in range(NS)]
    attT_d = [kb.dram("attT%d" % s, [D, TL], BF16, okind) for s in range(NS)]

    PSB = [kb.ps([128, 512], F32, "bank") for _ in range(8)]
    pstate = {"i": 0}

    def psum():
        b = PSB[pstate["i"] % 8]
        pstate["i"] += 1
        return b

    ident = kb.sb([128, 128], F32, "ident")
    identb = kb.sb([128, 128], BF16, "identb")
    ones = kb.sb([128, 128], F32, "ones")
    kb.dma("sp", ident[:], ident_d[:], [ident_d], [ident])
    kb.op("dve", lambda e: e.tensor_copy(out=identb[:], in_=ident[:]), [ident], [identb])
    kb.op("dve", lambda e: e.memset(ones[:], 1.0), [], [ones])

    rr = {"i": 0}

    def evac_eng():
        rr["i"] += 1
        return "act" if rr["i"] % 2 == 0 else "dve"

    def copy(eng, out_b, out_ap, in_b, in_ap):
        if eng == "act":
            kb.op("act", lambda e: e.copy(out=out_ap, in_=in_ap), [in_b], [out_b])
        elif eng == "dve":
            kb.op("dve", lambda e: e.tensor_copy(out=out_ap, in_=in_ap), [in_b], [out_b])
        else:
            kb.op("pool", lambda e: e.tensor_copy(out=out_ap, in_=in_ap), [in_b], [out_b])

    cv = kb.sb([128, KC, NV], F32, "cv")
    kb.dma("sp", cv[:], cv_d[:], [cv_d], [cv])
    kb.op("act", lambda e: e.activation(out=cv[:], in_=cv[:], func=AF.Silu), [cv], [cv])
    mT = [kb.sb([128, 48, NV], F32, "mT") for _ in range(2)]
    n1c = kb.sb([128, 2, KC], F32, "n1c")
    n2c = kb.sb([128, 2, KC], F32, "n2c")
    badac = kb.sb([128, 2, 48], F32, "badac")
    for l in range(2):
        kb.dma("sp", n1c[:, l, :], n1_d[l], [n1_d], [n1c])
        kb.dma("sp", n2c[:, l, :], n2_d[l], [n2_d], [n2c])
        kb.dma("sp", badac[:, l, :], bada_d[l], [bada_d], [badac])
    Ac = kb.sb([128, 2, 2, NV, KC], F32, "Ac")
    Bc = kb.sb([128, 2, 2, NV, KC], F32, "Bc")
    diag = kb.sb([128, 128], F32, "diag")
    kb.push()
    wst = [kb.sb([128, KC, 256], F32, "wst") for _ in range(2)]
    wsti = {"i": 0}

    def stage():
        b = wst[wsti["i"] % 2]
        wsti["i"] += 1
        return b

    for l in range(2):
        for g in range(24):
            st = stage()
            kb.dma("sp", st[:], wada_d[l, :, :, g * 256:(g + 1) * 256], [wada_d], [st])
            pb = psum()
            for jj in range(2):
                for kc in range(KC):
                    kb.op("pe", lambda e, jj=jj, kc=kc: e.matmul(
                        pb[:, jj * NV:(jj + 1) * NV], lhsT=st[:, kc, jj * 128:(jj + 1) * 128], rhs=cv[:, kc, :],
                        start=(kc == 0), stop=(kc == KC - 1)), [st, cv], [pb], inc=(kc == KC - 1))
            for jj in range(2):
                ch = g * 2 + jj
                kb.op("dve", lambda e, jj=jj, ch=ch: e.tensor_scalar(
                    out=mT[l][:, ch, :], in0=pb[:, jj * NV:(jj + 1) * NV], scalar1=badac[:, l, ch:ch + 1], scalar2=None,
                    op0=ALU.add), [pb, badac], [mT[l]])
    kb.pop()
    for l in range(2):
        for sub in range(2):
            nrm = n1c if sub == 0 else n2c
            for v in range(NV):
                sh = mT[l][:, sub * 24 + 0:sub * 24 + 8, v]
                sc = mT[l][:, sub * 24 + 8:sub * 24 + 16, v]
                kb.op("dve", lambda e, sc=sc, l=l, sub=sub, v=v, nrm=nrm: e.scalar_tensor_tensor(
                    out=Ac[:, l, sub, v, :], in0=sc, scalar=1.0, in1=nrm[:, l, :], op0=ALU.add, op1=ALU.mult),
                    [mT[l], nrm], [Ac])
                kb.op("dve", lambda e, sh=sh, l=l, sub=sub, v=v: e.tensor_copy(out=Bc[:, l, sub, v, :], in_=sh),
                      [mT[l]], [Bc])

    def make_grow(l, sub, v):
        gb = kb.sb([128, D], F32, "G")
        for half in range(2):
            pb = psum()
            for jj in range(4):
                j = half * 4 + jj
                col = mT[l][:, sub * 24 + 16 + j, v:v + 1]
                kb.op("dve", lambda e, col=col: e.tensor_scalar(
                    out=diag[:], in0=ident[:], scalar1=col, scalar2=None, op0=ALU.mult),
                    [ident, mT[l]], [diag])
                kb.op("pe", lambda e, jj=jj, pb=pb: e.matmul(pb[:, jj * 128:(jj + 1) * 128], lhsT=ones[:], rhs=diag[:],
                                                       start=True, stop=True), [ones, diag], [pb])
            copy("act", gb, gb[:, half * 512:(half + 1) * 512], pb, pb[:])
        return gb

    gdram = {}
    kb.push()
    for l in range(2):
        for sub in range(2):
            for v in range(NV):
                if v == NS and l == 1:
                    continue
                gb = make_grow(l, sub, v)
                gd = kb.dram("gd_%d_%d_%d" % (l, sub, v), [128, D], F32)
                gdram[(l, sub, v)] = gd
                kb.dma("pool", gd[:], gb[:], [gb], [gd])
    kb.pop()

    stat = [kb.sb([128, 4], F32, "stat") for _ in range(4)]
    xn_b = [kb.sb([128, D], F32, "xn") for _ in range(2)]
    nm = {"i": 0}

    def norm_mod_T(xb, x_ap, l, sub, v, hT, hT_ap_fn, h32=None):
        i = nm["i"]
        nm["i"] += 1
        stt = stat[i % 4]
        xn = xn_b[i % 2]
        kb.op("act", lambda e: e.activation(out=xn[:], in_=x_ap, func=AF.Square, accum_out=stt[:, 0:1]),
              [xb], [xn, stt])
        kb.op("dve", lambda e: e.tensor_scalar(out=stt[:, 1:2], in0=stt[:, 0:1], scalar1=1.0 / D, scalar2=EPS,
                                                op0=ALU.mult, op1=ALU.add), [stt], [stt])
        kb.op("act", lambda e: e.activation(out=stt[:, 2:3], in_=stt[:, 1:2], func=AF.Sqrt), [stt], [stt])
        kb.op("dve", lambda e: e.reciprocal(out=stt[:, 3:4], in_=stt[:, 2:3]), [stt], [stt])
        kb.op("dve", lambda e: e.tensor_scalar(out=xn[:], in0=x_ap, scalar1=stt[:, 3:4], scalar2=None, op0=ALU.mult),
              [xb, stt], [xn])
        for half in range(2):
            pb = psum()
            for jj in range(4):
                j = half * 4 + jj
                kb.op("pe", lambda e, jj=jj, j=j, pb=pb: e.transpose(out=pb[:, jj * 128:(jj + 1) * 128],
                                                                     in_=xn[:, j * 128:(j + 1) * 128], identity=ident[:]),
                      [xn, ident], [pb])
            for jj in range(4):
                j = half * 4 + jj
                kb.op("act", lambda e, jj=jj, j=j: e.activation(
                    out=hT_ap_fn(j), in_=pb[:, jj * 128:(jj + 1) * 128], func=AF.Identity,
                    scale=Ac[:, l, sub, v, j:j + 1], bias=Bc[:, l, sub, v, j:j + 1]), [pb, Ac, Bc], [hT])
                if h32 is not None:
                    kb.op("act", lambda e, jj=jj, j=j: e.activation(
                        out=h32[:, j, :], in_=pb[:, jj * 128:(jj + 1) * 128], func=AF.Identity,
                        scale=Ac[:, l, sub, v, j:j + 1], bias=Bc[:, l, sub, v, j:j + 1]), [pb, Ac, Bc], [h32])

    def tile_src(s, t0):
        if t0 < TC:
            return ctx_d, ctx_d[s, t0:t0 + 128, :]
        return x_d, x_d[s, t0 - TC:t0 - TC + 128, :]

    def load_w_bf16(dst, dst_ap_fn, src_b, src_ap_fn, ncols, step=512):
        for c0 in range(0, ncols, step):
            n = min(step, ncols - c0)
            kb.dma("pool", dst_ap_fn(c0, n), src_ap_fn(c0, n), [src_b], [dst])

    groups = token_groups(TC, TL)
    kb.push()
    winb = kb.sb([128, KC, 2592], BF16, "winb")
    load_w_bf16(winb, lambda c0, n: winb[:, :, c0:c0 + n], win_d, lambda c0, n: win_d[:, :, c0:c0 + n], 2592)
    xt_b = [kb.sb([128, D], F32, "xt") for _ in range(3)]
    hTg = [kb.sb([128, KC, 512], BF16, "hTg") for _ in range(3)]
    fst = [kb.sb([128, 512], F32, "fst") for _ in range(4)]
    vst = [kb.sb([128, 512], BF16, "vst") for _ in range(2)]
    cnt = {"x": 0, "g": 0, "f": 0, "v": 0}
    fchunks = []
    for cc in range(4):
        fchunks.append((cc * 128, 128, xaT_d, cc * 128))
    for cc in range(4):
        fchunks.append((512 + cc * 128, 128, gaT_d, cc * 128))
    for cc in range(2):
        fchunks.append((1024 + cc * 128, 128, qT_d, cc * 128))
    for cc in range(2):
        fchunks.append((1280 + cc * 128, 128, kT_d, cc * 128))
    def p0_task(s, t0, n):
            v = NS if t0 < TC else s
            hT = hTg[cnt["g"] % 3]
            cnt["g"] += 1
            for i in range(n // 128):
                xt = xt_b[cnt["x"] % 3]
                cnt["x"] += 1
                sb_, sap = tile_src(s, t0 + i * 128)
                kb.dma("sp", xt[:], sap, [sb_], [xt])
                norm_mod_T(xt, xt[:], 0, 0, v, hT, lambda j, i=i, hT=hT: hT[:, j, i * 128:(i + 1) * 128])
            yield
            for (c0, M, dst, r0) in fchunks:
                pb = psum()
                for kc in range(KC):
                    kb.op("pe", lambda e, kc=kc, c0=c0, M=M, pb=pb: e.matmul(
                        pb[0:M, 0:n], lhsT=winb[:, kc, c0:c0 + M], rhs=hT[:, kc, 0:n],
                        start=(kc == 0), stop=(kc == KC - 1)), [winb, hT], [pb], inc=(kc == KC - 1))
                st = fst[cnt["f"] % 4]
                cnt["f"] += 1
                copy(evac_eng(), st, st[0:M, 0:n], pb, pb[0:M, 0:n])
                kb.dma("pool", dst[s][r0:r0 + M, t0:t0 + n], st[0:M, 0:n], [st], [dst[s]])
            for dd in range(2):
                pb = psum()
                c0 = 2560 + dd * 16
                for kc in range(KC):
                    kb.op("pe", lambda e, kc=kc, c0=c0, pb=pb: e.matmul(
                        pb[0:16, 0:n], lhsT=winb[:, kc, c0:c0 + 16], rhs=hT[:, kc, 0:n],
                        start=(kc == 0), stop=(kc == KC - 1)), [winb, hT], [pb], inc=(kc == KC - 1))
                st = fst[cnt["f"] % 4]
                cnt["f"] += 1
                copy(evac_eng(), st, st[0:16, 0:n], pb, pb[0:16, 0:n])
                kb.dma("pool", lrT_d[s][dd, :, t0:t0 + n], st[0:16, 0:n], [st], [lrT_d[s]])
            yield
            for i in range(n // 128):
                tt = t0 + i * 128
                for which in range(2):
                    c0 = 1536 + which * 512
                    pb = psum()
                    for kc in range(KC):
                        kb.op("pe", lambda e, kc=kc, c0=c0, pb=pb, i=i: e.matmul(
                            pb[:, :], lhsT=hT[:, kc, i * 128:(i + 1) * 128], rhs=winb[:, kc, c0:c0 + 512],
                            start=(kc == 0), stop=(kc == KC - 1)), [winb, hT], [pb], inc=(kc == KC - 1))
                    if which == 0:
                        st = vst[cnt["v"] % 2]
                        cnt["v"] += 1
                        copy(evac_eng(), st, st[:], pb, pb[:])
                        kb.dma("pool", v_d[s][tt:tt + 128, :], st[:], [st], [v_d[s]])
                    else:
                        st = fst[cnt["f"] % 4]
                        cnt["f"] += 1
                        copy(evac_eng(), st, st[:], pb, pb[:])
                        kb.dma("pool", r_d[s][tt:tt + 128, :], st[:], [st], [r_d[s]])

    run_pipeline((p0_task(s, t0, n) for s in range(NS) for (t0, n) in groups), 3)

    def finish():
        print("KB: ninst at finish", kb.ninst, "stop", STOP)
        if kb.phase is not None:
            kb.pop()
        else:
            kb.barrier()
        es.close()
        return nc

    kb.pop()
    if STOP == "P0":
        return finish()

    def ps16(pb):
        return pb[:].bitcast(BF16)

    kb.push()
    cw = kb.sb([128, 4, 4], F32, "cw")
    cb = kb.sb([128, 4], F32, "cb")
    lba = kb.sb([128, 2, 4], F32, "lba")
    lbi = kb.sb([128, 2, 4], F32, "lbi")
    lam = kb.sb([128, 2, 4], F32, "lam")
    cl1 = kb.sb([128, 2, 4], F32, "cl1")
    cl2 = kb.sb([128, 2, 4], F32, "cl2")
    kb.dma("sp", cw[:], convw_d[:], [convw_d], [cw])
    kb.dma("sp", cb[:], convb_d[:], [convb_d], [cb])
    kb.dma("sp", lba[:], lba_d[:], [lba_d], [lba])
    kb.dma("sp", lbi[:], lbi_d[:], [lbi_d], [lbi])
    kb.dma("sp", lam[:], llam_d[:], [llam_d], [lam])
    kb.op("act", lambda e: e.activation(out=lam[:], in_=lam[:], func=AF.Exp, scale=-1.0), [lam], [lam])
    kb.op("act", lambda e: e.activation(out=lam[:], in_=lam[:], func=AF.Ln, bias=1.0), [lam], [lam])
    kb.op("dve", lambda e: e.tensor_scalar(out=cl1[:], in0=lam[:], scalar1=-8.0, scalar2=None, op0=ALU.mult), [lam], [cl1])
    kb.op("dve", lambda e: e.tensor_scalar(out=cl2[:], in0=lam[:], scalar1=-16.0, scalar2=None, op0=ALU.mult), [lam], [cl2])
    lw = kb.sb([128, 2, 8, 128], BF16, "lw")
    lst = [kb.sb([128, KC, 128], F32, "lst") for _ in range(2)]
    for ai, wd in enumerate((lwa_d, lwi_d)):
        st = lst[ai]
        stv = st[:].rearrange("p a b -> p (a b)")[:, 0:1024].rearrange("p (a b) -> p a b", b=128)
        kb.dma("sp", stv, wd[:].rearrange("d c k n -> k (d c) n"), [wd], [st])
        kb.op("pool", lambda e, ai=ai, stv=stv: e.tensor_copy(out=lw[:, ai, :, :], in_=stv), [st], [lw])
    BX = kb.sb([128, T], F32, "BX")
    BU = kb.sb([128, T], F32, "BU")
    BUB = kb.sb([128, T], BF16, "BUB")
    BR = kb.sb([128, T], F32, "BR")
    BI = kb.sb([128, T], F32, "BI")
    BHS = kb.sb([128, T], F32, "BHS")
    BH = kb.sb([128, T], F32, "BH")
    BY = kb.sb([128, T], BF16, "BY")
    g512 = [(t0, min(512, T - t0)) for t0 in range(0, T, 512)]
    segs = [(0, TC), (TC, T)]
    for s in range(NS):
        for cc in range(4):
            kb.dma("sp", BX[:], xaT_d[s][cc * 128:(cc + 1) * 128, :], [xaT_d[s]], [BX])
            kb.op("dve", lambda e, cc=cc: e.tensor_scalar(out=BU[:], in0=BX[:], scalar1=cw[:, cc, 2:3],
                                                          scalar2=cb[:, cc:cc + 1], op0=ALU.mult, op1=ALU.add),
                  [BX, cw, cb], [BU])
            for (s0, s1) in segs:
                for j in (0, 1, 3):
                    o = j - 2
                    lo = max(s0, s0 - o)
                    hi = min(s1, s1 - o)
                    kb.op("dve", lambda e, cc=cc, j=j, lo=lo, hi=hi, o=o: e.scalar_tensor_tensor(
                        out=BU[:, lo:hi], in0=BX[:, lo + o:hi + o], scalar=cw[:, cc, j:j + 1], in1=BU[:, lo:hi],
                        op0=ALU.mult, op1=ALU.add), [BX, cw, BU], [BU])
            kb.op("act", lambda e: e.copy(out=BUB[:], in_=BU[:]), [BU], [BUB])
            for d in range(2):
                for (t0, n) in g512:
                    for ai, dst, bb in ((0, BR, lba), (1, BI, lbi)):
                        pb = psum()
                        kb.op("pe", lambda e, ai=ai, pb=pb, t0=t0, n=n, d=d, cc=cc: e.matmul(
                            pb[:, 0:n], lhsT=lw[:, ai, d * 4 + cc, :], rhs=BUB[:, t0:t0 + n], start=True, stop=True),
                            [lw, BUB], [pb])
                        kb.op("act", lambda e, pb=pb, dst=dst, bb=bb, t0=t0, n=n, d=d, cc=cc: e.activation(
                            out=dst[:, t0:t0 + n], in_=pb[:, 0:n], func=AF.Sigmoid, bias=bb[:, d, cc:cc + 1]),
                            [pb, bb], [dst])
                kb.op("act", lambda e, d=d, cc=cc: e.activation(out=BX[:], in_=BR[:], func=AF.Exp,
                                                                scale=cl1[:, d, cc:cc + 1]), [BR, cl1], [BX])
                kb.op("act", lambda e, d=d, cc=cc: e.activation(out=BR[:], in_=BR[:], func=AF.Exp,
                                                                scale=cl2[:, d, cc:cc + 1]), [BR, cl2], [BR])
                kb.op("act", lambda e: e.activation(out=BR[:], in_=BR[:], func=AF.Sqrt, scale=-1.0, bias=1.0),
                      [BR], [BR])
                kb.op("dve", lambda e: e.tensor_tensor(out=BI[:], in0=BI[:], in1=BR[:], op=ALU.mult), [BI, BR], [BI])
                kb.op("pool", lambda e: e.tensor_tensor(out=BI[:], in0=BI[:], in1=BU[:], op=ALU.mult), [BI, BU], [BI])
                H = BHS if d == 0 else BH
                if d == 0:
                    kb.op("dve", lambda e, H=H: e.tensor_tensor_scan(
                        out=H[:, 0:TC], data0=BX[:, 0:TC], data1=BI[:, 0:TC], initial=0.0, op0=ALU.mult, op1=ALU.add),
                        [BX, BI], [H])
                    kb.op("dve", lambda e, H=H: e.tensor_tensor_scan(
                        out=H[:, TC:T], data0=BX[:, TC:T], data1=BI[:, TC:T], initial=H[:, TC - 1:TC],
                        op0=ALU.mult, op1=ALU.add), [BX, BI, H], [H])
                else:
                    kb.op("dve", lambda e, H=H: e.tensor_tensor_scan(
                        out=H[:, 0:TC][:, ::-1], data0=BX[:, 0:TC][:, ::-1], data1=BI[:, 0:TC][:, ::-1], initial=0.0,
                        op0=ALU.mult, op1=ALU.add), [BX, BI], [H])
                    kb.op("dve", lambda e, H=H: e.tensor_tensor_scan(
                        out=H[:, TC:T][:, ::-1], data0=BX[:, TC:T][:, ::-1], data1=BI[:, TC:T][:, ::-1],
                        initial=H[:, 0:1], op0=ALU.mult, op1=ALU.add), [BX, BI, H], [H])
            kb.dma("sp", BR[:], gaT_d[s][cc * 128:(cc + 1) * 128, :], [gaT_d[s]], [BR])
            kb.op("act", lambda e: e.activation(out=BR[:], in_=BR[:], func=AF.Gelu_apprx_tanh), [BR], [BR])
            kb.op("pool", lambda e: e.tensor_tensor(out=BHS[:], in0=BHS[:], in1=BH[:], op=ALU.add), [BHS, BH], [BHS])
            kb.op("dve", lambda e: e.tensor_tensor(out=BY[:], in0=BHS[:], in1=BR[:], op=ALU.mult), [BHS, BR], [BY])
            kb.dma("pool", mixT_d[s][cc * 128:(cc + 1) * 128, :], BY[:], [BY], [mixT_d[s]])
    kb.pop()
    if STOP == "M0a":
        return finish()

    kb.push()
    maskf = kb.sb([128, 128], F32, "maskf")
    maskb = kb.sb([128, 128], F32, "maskb")
    kb.dma("sp", maskf[:], maskf_d[:], [maskf_d], [maskf])
    kb.dma("sp", maskb[:], maskb_d[:], [maskb_d], [maskb])
    masks = (maskf, maskb)
    rm = [kb.sb([128, 512], F32, "rm") for _ in range(2)]
    for d in range(2):
        kb.op("pool", lambda e, d=d: e.memset(rm[d][:], 1.0), [], [rm[d]])
        off = 0 if d == 0 else 127
        kb.op("pool", lambda e, d=d, off=off: e.memset(rm[d][:, off::128], 0.0), [], [rm[d]])
    wg = kb.sb([16, 2, 256], F32, "wg")
    kb.dma("sp", wg[:], gwg_d[:].rearrange("d k n -> k d n"), [gwg_d], [wg])
    nbg = kb.sb([128, 2, 2], F32, "nbg")
    kb.dma("sp", nbg[:], gbg_d[:], [gbg_d], [nbg])
    kb.op("dve", lambda e: e.tensor_scalar(out=nbg[:], in0=nbg[:], scalar1=-1.0, scalar2=None, op0=ALU.mult), [nbg], [nbg])
    gnb = kb.sb([128, 128], F32, "gnb")
    kb.dma("sp", gnb[:], bass.AP(gnorm_d[:].tensor, 0, [[0, 128], [1, 128]]), [gnorm_d], [gnb])
    vsb = kb.sb([128, NT, 512], BF16, "vsb")
    qg = [[kb.sb([128, T], BF16, "qg") for hp in range(2)] for d in range(2)]
    kg = [[kb.sb([128, T], BF16, "kg") for hp in range(2)] for d in range(2)]
    dec = [[kb.sb([128, NT], F32, "dec") for hp in range(2)] for d in range(2)]
    Sf = [[kb.sb([128, 256], F32, "Sf") for hp in range(2)] for d in range(2)]
    Sb = [[kb.sb([128, 256], BF16, "Sb") for hp in range(2)] for d in range(2)]
    kgm = [kb.sb([128, 128], BF16, "kgm") for _ in range(8)]
    mcol = kb.sb([128, 2], F32, "mcol")
    bm = kb.sb([128, 256], F32, "bm")
    kb.op("pool", lambda e: e.memset(mcol[:], 0.0), [], [mcol])
    kb.op("pool", lambda e: e.memset(mcol[0:64, 0:1], 1.0), [], [mcol])
    kb.op("pool", lambda e: e.memset(mcol[64:128, 1:2], 1.0), [], [mcol])
    kb.op("pool", lambda e: e.memset(bm[:], 0.0), [], [bm])
    kb.op("pool", lambda e: e.memset(bm[0:64, 0:128], 1.0), [], [bm])
    kb.op("pool", lambda e: e.memset(bm[64:128, 128:256], 1.0), [], [bm])
    qgt = [kb.sb([128, 512], F32, "qgt") for _ in range(2)]
    kgt = [kb.sb([128, 512], F32, "kgt") for _ in range(2)]
    lrg = [kb.sb([16, 2, 512], F32, "lrg") for _ in range(2)]
    zt = [kb.sb([128, 512], F32, "zt") for _ in range(2)]
    Gt = [kb.sb([128, 512], F32, "Gt") for _ in range(2)]
    kdT = [kb.sb([128, 128], BF16, "kdT") for _ in range(4)]
    kdtok = [kb.sb([128, 256], BF16, "kdtok") for _ in range(2)]
    attsb = [kb.sb([128, 4, 128], BF16, "attsb") for _ in range(2)]
    ost = [kb.sb([128, 512], F32, "ost") for _ in range(2)]
    ofl = [kb.sb([128, 512], F32, "ofl") for _ in range(2)]
    rtl = [kb.sb([128, 512], F32, "rtl") for _ in range(2)]
    osq = kb.sb([128, 512], F32, "osq")
    gst = [kb.sb([128, 8], F32, "gst") for _ in range(2)]
    ybb = [kb.sb([128, 512], BF16, "ybb") for _ in range(2)]
    ybT = [kb.sb([128, 4, 128], BF16, "ybT") for _ in range(2)]
    gc = {"g": 0, "k": 0, "c": 0, "f": 0}
    for s in range(NS):
        kb.dma("sp", vsb[:], v_d[s][:].rearrange("(n p) c -> p n c", p=128), [v_d[s]], [vsb])
        for (t0, n) in g512:
            i = gc["g"] % 2
            gc["g"] += 1
            kb.dma("sp", lrg[i][:, :, 0:n], lrT_d[s][:, :, t0:t0 + n].rearrange("d k t -> k d t"), [lrT_d[s]], [lrg[i]])
            for hp in range(2):
                j = gc["k"] % 2
                gc["k"] += 1
                kb.dma("sp", qgt[j][:, 0:n], qT_d[s][hp * 128:(hp + 1) * 128, t0:t0 + n], [qT_d[s]], [qgt[j]])
                kb.dma("sp", kgt[j][:, 0:n], kT_d[s][hp * 128:(hp + 1) * 128, t0:t0 + n], [kT_d[s]], [kgt[j]])
                for d in range(2):
                    z = zt[d]
                    G = Gt[d]
                    pb = psum()
                    kb.op("pe", lambda e, pb=pb, d=d, hp=hp, i=i, n=n: e.matmul(
                        pb[:, 0:n], lhsT=wg[:, d, hp * 128:(hp + 1) * 128], rhs=lrg[i][:, d, 0:n], start=True, stop=True),
                        [wg, lrg[i]], [pb])
                    kb.op("act", lambda e, pb=pb, z=z, d=d, hp=hp, n=n: e.activation(
                        out=z[:, 0:n], in_=pb[:, 0:n], func=AF.Exp, scale=-1.0, bias=nbg[:, d, hp:hp + 1]), [pb, nbg], [z])
                    kb.op("act", lambda e, z=z, n=n: e.activation(out=z[:, 0:n], in_=z[:, 0:n], func=AF.Ln, bias=1.0),
                          [z], [z])
                    if d == 0:
                        kb.op("dve", lambda e, z=z, G=G, n=n: e.tensor_tensor_scan(
                            out=G[:, 0:n], data0=rm[0][:, 0:n], data1=z[:, 0:n], initial=0.0, op0=ALU.mult, op1=ALU.add),
                            [rm[0], z], [G])
                    else:
                        kb.op("dve", lambda e, z=z, G=G, n=n: e.tensor_tensor_scan(
                            out=G[:, 0:n][:, ::-1], data0=rm[1][:, 0:n][:, ::-1], data1=z[:, 0:n][:, ::-1], initial=0.0,
                            op0=ALU.mult, op1=ALU.add), [rm[1], z], [G])
                    kb.op("act", lambda e, z=z, G=G, n=n: e.activation(out=z[:, 0:n], in_=G[:, 0:n], func=AF.Exp,
                                                                       scale=-1.0 / 16), [G], [z])
                    kb.op("dve", lambda e, z=z, j=j, d=d, hp=hp, t0=t0, n=n: e.scalar_tensor_tensor(
                        out=qg[d][hp][:, t0:t0 + n], in0=qgt[j][:, 0:n], scalar=0.125, in1=z[:, 0:n],
                        op0=ALU.mult, op1=ALU.mult), [qgt[j], z], [qg[d][hp]])
                    offl = 127 if d == 0 else 0
                    kb.op("pool", lambda e, z=z, d=d, hp=hp, t0=t0, n=n, offl=offl: e.tensor_copy(
                        out=dec[d][hp][:, t0 // 128:(t0 + n) // 128], in_=z[:, offl:n:128]), [z], [dec[d][hp]])
                    kb.op("act", lambda e, G=G, n=n: e.activation(out=G[:, 0:n], in_=G[:, 0:n], func=AF.Exp,
                                                                  scale=1.0 / 16), [G], [G])
                    kb.op("dve", lambda e, G=G, j=j, d=d, hp=hp, t0=t0, n=n: e.tensor_tensor(
                        out=kg[d][hp][:, t0:t0 + n], in0=kgt[j][:, 0:n], in1=G[:, 0:n], op=ALU.mult),
                        [kgt[j], G], [kg[d][hp]])
        print("KB: mark prep-done", kb.ninst)
        order = [list(range(NT)), list(range(NTC - 1, -1, -1)) + list(range(NT - 1, NTC - 1, -1))]
        for d in range(2):
            for hp in range(2):
                kb.op("pool", lambda e, d=d, hp=hp: e.memset(Sf[d][hp][:], 0.0), [], [Sf[d][hp]])
                kb.op("pool", lambda e, d=d, hp=hp: e.memset(Sb[d][hp][:], 0.0), [], [Sb[d][hp]])
        seen_c = set()
        for step in range(NT):
            for d in range(2):
                c = order[d][step]
                cs = slice(c * 128, (c + 1) * 128)
                if step < 2:
                    print("KB: mark step", step, d, kb.ninst)
                ci = gc["c"] % 2
                gc["c"] += 1
                pT = psum()
                pT16 = ps16(pT)
                for hp in range(2):
                    kt = kdT[(gc["c"] * 2 + hp) % 4]
                    kb.op("dve", lambda e, kt=kt, d=d, hp=hp, cs=cs, c=c: e.tensor_scalar(
                        out=kt[:], in0=kg[d][hp][:, cs], scalar1=dec[d][hp][:, c:c + 1], scalar2=None, op0=ALU.mult),
                        [kg[d][hp], dec[d][hp]], [kt])
                    kb.op("pe", lambda e, kt=kt, hp=hp, pT16=pT16: e.transpose(
                        out=pT16[:, hp * 128:(hp + 1) * 128], in_=kt[:], identity=identb[:]), [kt, identb], [pT])
                kdk = kdtok[ci]
                kb.op("act", lambda e, kdk=kdk, pT16=pT16: e.copy(out=kdk[:], in_=pT16[:, 0:256]), [pT], [kdk])
                pA = psum()
                for head in range(4):
                    hp, h = head // 2, head % 2
                    km = kgm[(gc["c"] * 4 + head) % 8]
                    kb.op("dve", lambda e, km=km, d=d, hp=hp, h=h, cs=cs: e.tensor_scalar(
                        out=km[:], in0=kg[d][hp][:, cs], scalar1=mcol[:, h:h + 1], scalar2=None, op0=ALU.mult),
                        [kg[d][hp], mcol], [km])
                    kb.op("pe", lambda e, pA=pA, head=head, hp=hp, km=km, d=d, cs=cs: e.matmul(
                        pA[:, head * 128:(head + 1) * 128], lhsT=km[:], rhs=qg[d][hp][:, cs],
                        start=True, stop=True), [km, qg[d][hp]], [pA])
                asb = attsb[ci]
                mk = masks[d]
                kb.op("dve", lambda e, asb=asb, pA=pA, mk=mk: e.tensor_tensor(
                    out=asb[:], in0=pA[:].rearrange("p (h i) -> p h i", h=4),
                    in1=bass.AP(mk[:].tensor, mk[:].offset, [list(mk[:].ap[0]), [0, 4], [1, 128]]), op=ALU.mult),
                    [pA, mk], [asb])
                pO = psum()
                pD = psum()
                for hp in range(2):
                    pc = slice(hp * 256, (hp + 1) * 256)
                    kb.op("pe", lambda e, pO=pO, hp=hp, pc=pc, d=d, cs=cs: e.matmul(
                        pO[:, pc], lhsT=qg[d][hp][:, cs], rhs=Sb[d][hp][:], start=True, stop=False),
                        [qg[d][hp], Sb[d][hp]], [pO], inc=False)
                    for h in range(2):
                        head = hp * 2 + h
                        hc = slice(head * 128, (head + 1) * 128)
                        kb.op("pe", lambda e, pO=pO, asb=asb, head=head, hc=hc, c=c, h=h: e.matmul(
                            pO[:, hc], lhsT=asb[:, head, :], rhs=vsb[:, c, hc], start=False, stop=(h == 1)),
                            [asb, vsb], [pO], inc=(h == 1))
                for hp in range(2):
                    pc = slice(hp * 256, (hp + 1) * 256)
                    kb.op("pe", lambda e, pD=pD, kdk=kdk, hp=hp, pc=pc, c=c: e.matmul(
                        pD[:, pc], lhsT=kdk[:, hp * 128:(hp + 1) * 128], rhs=vsb[:, c, pc], start=True, stop=True),
                        [kdk, vsb], [pD])
                for hp in range(2):
                    kb.op("dve", lambda e, pD=pD, d=d, hp=hp, c=c: e.scalar_tensor_tensor(
                        out=Sf[d][hp][:], in0=Sf[d][hp][:], scalar=dec[d][hp][:, c:c + 1],
                        in1=pD[:, hp * 256:(hp + 1) * 256], op0=ALU.mult, op1=ALU.add),
                        [Sf[d][hp], dec[d][hp], pD], [Sf[d][hp]])
                    kb.op("dve", lambda e, d=d, hp=hp: e.tensor_tensor(out=Sb[d][hp][:], in0=Sf[d][hp][:], in1=bm[:],
                                                                       op=ALU.mult), [Sf[d][hp], bm], [Sb[d][hp]])
                if c not in seen_c:
                    seen_c.add(c)
                    o1 = ost[ci]
                    kb.op("act", lambda e, o1=o1, pO=pO: e.copy(out=o1[:], in_=pO[:]), [pO], [o1])
                    kb.dma("pool", of_d[s][cs, :], o1[:], [o1], [of_d[s]])
                else:
                    fi = gc["f"] % 2
                    gc["f"] += 1
                    o2 = ofl[fi]
                    rt = rtl[fi]
                    gs = gst[fi]
                    kb.dma("sp", o2[:], of_d[s][cs, :], [of_d[s]], [o2])
                    kb.dma("sp", rt[:], r_d[s][cs, :], [r_d[s]], [rt])
                    kb.op("dve", lambda e, o2=o2, pO=pO: e.tensor_tensor(out=o2[:], in0=pO[:], in1=o2[:], op=ALU.add),
                          [pO, o2], [o2])
                    kb.op("act", lambda e, o2=o2: e.activation(out=osq[:], in_=o2[:], func=AF.Square), [o2], [osq])
                    kb.op("dve", lambda e, gs=gs: e.tensor_reduce(
                        out=gs[:, 0:4], in_=osq[:].rearrange("p (h e) -> p h e", h=4), axis=AX.X, op=ALU.add), [osq], [gs])
                    kb.op("dve", lambda e, gs=gs: e.tensor_scalar(out=gs[:, 0:4], in0=gs[:, 0:4], scalar1=1.0 / 128,
                                                                  scalar2=EPS, op0=ALU.mult, op1=ALU.add), [gs], [gs])
                    kb.op("act", lambda e, gs=gs: e.activation(out=gs[:, 0:4], in_=gs[:, 0:4], func=AF.Sqrt), [gs], [gs])
                    kb.op("dve", lambda e, gs=gs: e.reciprocal(out=gs[:, 4:8], in_=gs[:, 0:4]), [gs], [gs])
                    kb.op("dve", lambda e, o2=o2, gs=gs: e.tensor_tensor(
                        out=o2[:].rearrange("p (h e) -> p h e", h=4), in0=o2[:].rearrange("p (h e) -> p h e", h=4),
                        in1=bass.AP(gs[:].tensor, gs[:].offset + 4, [list(gs[:].ap[0]), [1, 4], [0, 128]]), op=ALU.mult),
                        [o2, gs], [o2])
                    kb.op("pool", lambda e, o2=o2: e.tensor_tensor(
                        out=o2[:].rearrange("p (h e) -> p h e", h=4), in0=o2[:].rearrange("p (h e) -> p h e", h=4),
                        in1=bass.AP(gnb[:].tensor, gnb[:].offset, [list(gnb[:].ap[0]), [0, 4], [1, 128]]), op=ALU.mult),
                        [o2, gnb], [o2])
                    kb.op("act", lambda e, rt=rt: e.activation(out=rt[:], in_=rt[:], func=AF.Silu), [rt], [rt])
                    yb = ybb[fi]
                    kb.op("dve", lambda e, yb=yb, o2=o2, rt=rt: e.tensor_tensor(out=yb[:], in0=o2[:], in1=rt[:], op=ALU.mult),
                          [o2, rt], [yb])
                    pY = psum()
                    pY16 = ps16(pY)
                    for head in range(4):
                        kb.op("pe", lambda e, yb=yb, head=head, pY16=pY16: e.transpose(
                            out=pY16[:, head * 128:(head + 1) * 128], in_=yb[:, head * 128:(head + 1) * 128],
                            identity=identb[:]), [yb, identb], [pY], inc=(head == 3))
                    yT = ybT[fi]
                    kb.op("act", lambda e, yT=yT, pY16=pY16: e.copy(out=yT[:].rearrange("p h t -> p (h t)"), in_=pY16[:, 0:512]),
                          [pY], [yT])
                    kb.dma("pool", mixT_d[s][512:1024, cs].rearrange("(h p) t -> p h t", p=128), yT[:], [yT], [mixT_d[s]])
    kb.pop()
    if STOP == "M0":
        return finish()

    xp_d = [kb.dram("xp_%d" % s, [T, D], F32, okind) for s in range(NS)]

    def bc_rows(b_, n_mid, n_in):
        a_ = b_[:]
        return bass.AP(a_.tensor, a_.offset, [list(a_.ap[0]), [0, n_mid], [1, n_in]])

    def post_phase(l):
        kb.push()
        wpb = kb.sb([128, KC, D], BF16, "wpb")
        wsrc = wout_d if l == 0 else wo_d
        load_w_bf16(wpb, lambda c0, n: wpb[:, :, c0:c0 + n], wsrc, lambda c0, n: wsrc[:, :, c0:c0 + n], D)
        wr = kb.sb([128, KC, 20], F32, "wr")
        kb.dma("sp", wr[:], wr_d[l], [wr_d], [wr])
        brb = kb.sb([128, 20], F32, "brb")
        kb.dma("sp", brb[:], bass.AP(br_d[:].tensor, l * 20, [[0, 128], [1, 20]]), [br_d], [brb])
        H2 = kb.sb([128, KC, 1024], BF16, "H2")
        ACC = kb.sb([128, 8, D], F32, "ACC")
        COMB = kb.sb([128, 8, 16], F32, "COMB")
        wring = [kb.sb([128, 4096], BF16, "wring") for _ in range(4)]
        HE = [kb.sb([128, 4, 512], BF16, "HE") for _ in range(2)]
        SIL = [kb.sb([128, 512], F32, "SIL") for _ in range(2)]
        Mb = [kb.sb([128, KC, 128], BF16, "Mb") for _ in range(3)]
        xtb = [kb.sb([128, D], F32, "xtb") for _ in range(3)]
        gtb = [kb.sb([128, D], F32, "gtb") for _ in range(3)]
        tmpbs = [kb.sb([128, D], F32, "tmpb") for _ in range(2)]
        xpb = [kb.sb([128, D], F32, "xpb") for _ in range(3)]
        h32s = [kb.sb([128, KC, 128], F32, "h32") for _ in range(2)]
        rt = [kb.sb([128, 64], F32, "rt") for _ in range(3)]
        tiles = []
        for s in range(NS):
            for t0 in range(0 if l == 0 else TC, T, 128):
                tiles.append((s, t0))
        blocks = [tiles[i:i + 8] for i in range(0, len(tiles), 8)]
        wc = {"w": 0, "k": 0, "x": 0}
        def pre_tile(i, s, t0):
            for _once in (0,):
                v = NS if t0 < TC else s
                Mt = Mb[wc["x"] % 3]
                xt = xtb[wc["x"] % 3]
                g1 = gtb[wc["x"] % 3]
                xp = xpb[wc["x"] % 3]
                r_ = rt[wc["x"] % 3]
                tmpb = tmpbs[wc["x"] % 2]
                h32 = h32s[wc["x"] % 2]
                wc["x"] += 1
                if l == 0:
                    kb.dma("sp", Mt[:], mixT_d[s][:, t0:t0 + 128].rearrange("(k p) t -> p k t", p=128), [mixT_d[s]], [Mt])
                    sb_, sap = tile_src(s, t0)
                    kb.dma("sp", xt[:], sap, [sb_], [xt])
                else:
                    kb.dma("sp", Mt[:], attT_d[s][:, t0 - TC:t0 - TC + 128].rearrange("(k p) t -> p k t", p=128),
                           [attT_d[s]], [Mt])
                    kb.dma("sp", xt[:], x1_d[s][t0:t0 + 128, :], [x1_d[s]], [xt])
                kb.dma("sp", g1[:], gdram[(l, 0, v)][:], [gdram[(l, 0, v)]], [g1])
                for half in range(2):
                    pb = psum()
                    hs_ = slice(half * 512, (half + 1) * 512)
                    for kc in range(KC):
                        kb.op("pe", lambda e, pb=pb, kc=kc, Mt=Mt, hs_=hs_: e.matmul(
                            pb[:, :], lhsT=Mt[:, kc, :], rhs=wpb[:, kc, hs_], start=(kc == 0), stop=(kc == KC - 1)),
                            [Mt, wpb], [pb], inc=(kc == KC - 1))
                    kb.op("dve", lambda e, pb=pb, g1=g1, hs_=hs_: e.tensor_tensor(
                        out=tmpb[:, hs_], in0=pb[:, :], in1=g1[:, hs_], op=ALU.mult), [pb, g1], [tmpb])
                kb.op("dve", lambda e, xp=xp, xt=xt, tmpb=tmpb: e.tensor_tensor(out=xp[:], in0=tmpb[:], in1=xt[:], op=ALU.add),
                      [tmpb, xt], [xp])
                kb.dma("pool", xp_d[s][t0:t0 + 128, :], xp[:], [xp], [xp_d[s]])
                yield
                norm_mod_T(xp, xp[:], l, 1, v, H2, lambda j, i=i: H2[:, j, i * 128:(i + 1) * 128], h32=h32)
                yield
                pb = psum()
                for kc in range(KC):
                    kb.op("pe", lambda e, pb=pb, kc=kc: e.matmul(pb[:, 0:20], lhsT=h32[:, kc, :], rhs=wr[:, kc, :],
                                                                  start=(kc == 0), stop=(kc == KC - 1)),
                          [h32, wr], [pb], inc=(kc == KC - 1))
                dv = lambda fn, r_=r_: kb.op("dve", fn, [r_], [r_])
                kb.op("dve", lambda e, pb=pb, r_=r_: e.tensor_tensor(out=r_[:, 0:20], in0=pb[:, 0:20], in1=brb[:], op=ALU.add),
                      [pb, brb], [r_])
                dv(lambda e, r_=r_: e.tensor_reduce(out=r_[:, 20:21], in_=r_[:, 0:4], axis=AX.X, op=ALU.max))
                dv(lambda e, r_=r_: e.tensor_scalar(out=r_[:, 21:22], in0=r_[:, 20:21], scalar1=-1.0, scalar2=None, op0=ALU.mult))
                kb.op("act", lambda e, r_=r_: e.activation(out=r_[:, 24:28], in_=r_[:, 0:4], func=AF.Exp,
                                                           bias=r_[:, 21:22], accum_out=r_[:, 22:23]), [r_], [r_])
                dv(lambda e, r_=r_: e.reciprocal(out=r_[:, 23:24], in_=r_[:, 22:23]))
                dv(lambda e, r_=r_: e.tensor_scalar(out=r_[:, 24:28], in0=r_[:, 0:4], scalar1=r_[:, 20:21], scalar2=None,
                                                    op0=ALU.is_equal))
                dv(lambda e, r_=r_: e.tensor_scalar(out=r_[:, 24:28], in0=r_[:, 24:28], scalar1=BIG, scalar2=-BIG,
                                                    op0=ALU.mult, op1=ALU.add))
                dv(lambda e, r_=r_: e.tensor_tensor(
                    out=r_[:, 28:44].rearrange("p (g e) -> p g e", g=4), in0=r_[:, 4:20].rearrange("p (g e) -> p g e", g=4),
                    in1=bass.AP(r_[:].tensor, r_[:].offset + 24, [list(r_[:].ap[0]), [1, 4], [0, 4]]), op=ALU.add))
                dv(lambda e, r_=r_: e.max(out=r_[:, 44:52], in_=r_[:, 28:44]))
                dv(lambda e, r_=r_: e.tensor_tensor(out=r_[:, 52:53], in0=r_[:, 44:45], in1=r_[:, 45:46], op=ALU.subtract))
                kb.op("act", lambda e, r_=r_: e.activation(out=r_[:, 53:54], in_=r_[:, 52:53], func=AF.Sigmoid), [r_], [r_])
                kb.op("act", lambda e, r_=r_: e.activation(out=r_[:, 54:55], in_=r_[:, 52:53], func=AF.Sigmoid, scale=-1.0),
                      [r_], [r_])
                dv(lambda e, r_=r_: e.tensor_scalar(out=r_[:, 0:16], in0=r_[:, 28:44], scalar1=r_[:, 44:45], scalar2=r_[:, 53:54],
                                                    op0=ALU.is_equal, op1=ALU.mult))
                dv(lambda e, r_=r_: e.tensor_scalar(out=r_[:, 28:44], in0=r_[:, 28:44], scalar1=r_[:, 45:46], scalar2=r_[:, 54:55],
                                                    op0=ALU.is_equal, op1=ALU.mult))
                dv(lambda e, r_=r_: e.tensor_tensor(out=r_[:, 0:16], in0=r_[:, 0:16], in1=r_[:, 28:44], op=ALU.add))
                kb.op("dve", lambda e, r_=r_, i=i: e.tensor_scalar(out=COMB[:, i, :], in0=r_[:, 0:16], scalar1=r_[:, 23:24],
                                                                   scalar2=None, op0=ALU.mult), [r_], [COMB])
                yield

        def drain(g):
            for _ in g:
                pass

        for bi, blk in enumerate(blocks):
            nb = len(blk)
            if bi == 0:
                run_pipeline((pre_tile(i, s, t0) for i, (s, t0) in enumerate(blk)), 3)
            bg = None
            for ex in range(NEXP):
                wb = []
                for wi, wd in enumerate((w1_d, w3_d, w2_d)):
                    slot = wring[wc["w"] % 4]
                    wc["w"] += 1
                    wb.append(slot)
                    srcv = wd[l, ex].rearrange("p a b -> p (a b)")
                    for hh in range(2):
                        kb.dma("pool", slot[:, hh * 2048:(hh + 1) * 2048], srcv[:, hh * 2048:(hh + 1) * 2048],
                               [wd], [slot])
                W1 = wb[0][:].rearrange("p (k n) -> p k n", k=KC)
                W3 = wb[1][:].rearrange("p (k n) -> p k n", k=KC)
                W2 = wb[2][:].rearrange("p (k n) -> p k n", k=4)
                for g0 in range(0, nb, 4):
                    ntile = min(4, nb - g0)
                    n = ntile * 128
                    c0 = g0 * 128
                    he = HE[wc["k"] % 2]
                    wc["k"] += 1
                    for hc in range(4):
                        pb1 = psum()
                        pb3 = psum()
                        for (pbx, Wx, wbuf) in ((pb1, W1, wb[0]), (pb3, W3, wb[1])):
                            for kc in range(KC):
                                kb.op("pe", lambda e, pbx=pbx, Wx=Wx, kc=kc, hc=hc, c0=c0, n=n: e.matmul(
                                    pbx[:, 0:n], lhsT=Wx[:, kc, hc * 128:(hc + 1) * 128], rhs=H2[:, kc, c0:c0 + n],
                                    start=(kc == 0), stop=(kc == KC - 1)), [wbuf, H2], [pbx], inc=(kc == KC - 1))
                        sil = SIL[hc % 2]
                        kb.op("act", lambda e, sil=sil, pb1=pb1, n=n: e.activation(out=sil[:, 0:n], in_=pb1[:, 0:n], func=AF.Silu),
                              [pb1], [sil])
                        kb.op("dve", lambda e, he=he, hc=hc, sil=sil, pb3=pb3, n=n: e.tensor_tensor(
                            out=he[:, hc, 0:n], in0=sil[:, 0:n], in1=pb3[:, 0:n], op=ALU.mult), [sil, pb3], [he])
                    for ii in range(ntile):
                        i = g0 + ii
                        for half in range(2):
                            hs_ = slice(half * 512, (half + 1) * 512)
                            pbo = psum()
                            for hc in range(4):
                                kb.op("pe", lambda e, pbo=pbo, he=he, hc=hc, ii=ii, W2=W2, hs_=hs_: e.matmul(
                                    pbo[:, :], lhsT=he[:, hc, ii * 128:(ii + 1) * 128], rhs=W2[:, hc, hs_],
                                    start=(hc == 0), stop=(hc == 3)), [he, wb[2]], [pbo], inc=(hc == 3))
                            if ex == 0:
                                kb.op("dve", lambda e, pbo=pbo, i=i, hs_=hs_, ex=ex: e.tensor_scalar(
                                    out=ACC[:, i, hs_], in0=pbo[:, :], scalar1=COMB[:, i, ex:ex + 1], scalar2=None,
                                    op0=ALU.mult), [pbo, COMB], [ACC])
                            else:
                                kb.op("dve", lambda e, pbo=pbo, i=i, hs_=hs_, ex=ex: e.scalar_tensor_tensor(
                                    out=ACC[:, i, hs_], in0=pbo[:, :], scalar=COMB[:, i, ex:ex + 1], in1=ACC[:, i, hs_],
                                    op0=ALU.mult, op1=ALU.add), [pbo, COMB, ACC], [ACC])
                if bg is not None:
                    next(bg, None)
                    next(bg, None)
            if bg is not None:
                drain(bg)
            def fin_tile(i, s, t0):
                v = NS if t0 < TC else s
                xt = xtb[wc["x"] % 3]
                g2 = gtb[wc["x"] % 3]
                xo = xpb[wc["x"] % 3]
                wc["x"] += 1
                kb.dma("sp", xt[:], xp_d[s][t0:t0 + 128, :], [xp_d[s]], [xt])
                kb.dma("sp", g2[:], gdram[(l, 1, v)][:], [gdram[(l, 1, v)]], [g2])
                yield
                kb.op("dve", lambda e, g2=g2, i=i: e.tensor_tensor(out=g2[:], in0=ACC[:, i, :], in1=g2[:], op=ALU.mult),
                      [ACC, g2], [g2])
                kb.op("dve", lambda e, xo=xo, g2=g2, xt=xt: e.tensor_tensor(out=xo[:], in0=g2[:], in1=xt[:], op=ALU.add),
                      [g2, xt], [xo])
                if l == 0:
                    kb.dma("pool", x1_d[s][t0:t0 + 128, :], xo[:], [xo], [x1_d[s]])
                else:
                    kb.dma("pool", out_d[s, t0 - TC:t0 - TC + 128, :], xo[:], [xo], [out_d])

            tasks = [fin_tile(i, s, t0) for i, (s, t0) in enumerate(blk)]
            if bi + 1 < len(blocks):
                nxt = [pre_tile(i, s, t0) for i, (s, t0) in enumerate(blocks[bi + 1])]
                mixed = []
                for k_ in range(max(len(tasks), len(nxt))):
                    if k_ < len(tasks):
                        mixed.append(tasks[k_])
                    if k_ < len(nxt):
                        mixed.append(nxt[k_])
                tasks = mixed
            run_pipeline(iter(tasks), 4)
        kb.pop()

    post_phase(0)
    if STOP == "P1":
        return finish()

    kb.push()
    wqb = kb.sb([128, KC, 1536], BF16, "wqb")
    load_w_bf16(wqb, lambda c0, n: wqb[:, :, c0:c0 + n], wqkv_d, lambda c0, n: wqkv_d[:, :, c0:c0 + n], 1536)
    qrow = kb.sb([128, 128], F32, "qrow")
    krow = kb.sb([128, 128], F32, "krow")
    kb.dma("sp", qrow[:], bass.AP(qn_d[:].tensor, 0, [[0, 128], [1, 128]]), [qn_d], [qrow])
    kb.dma("sp", krow[:], bass.AP(kn_d[:].tensor, 0, [[0, 128], [1, 128]]), [kn_d], [krow])
    kb.op("dve", lambda e: e.tensor_scalar(out=qrow[:], in0=qrow[:], scalar1=128.0 ** -0.5, scalar2=None, op0=ALU.mult),
          [qrow], [qrow])
    xt1 = [kb.sb([128, D], F32, "xt1") for _ in range(3)]
    hT1 = [kb.sb([128, KC, 128], BF16, "hT1") for _ in range(3)]
    sqbs = [kb.sb([128, 512], F32, "sqb") for _ in range(2)]
    ssb = [kb.sb([128, 32], F32, "ssb") for _ in range(3)]
    qnb = [kb.sb([128, D], F32, "qnb") for _ in range(3)]
    knb = [kb.sb([128, 256], F32, "knb") for _ in range(3)]
    cosb = [kb.sb([128, 64], F32, "cosb") for _ in range(3)]
    sinb = [kb.sb([128, 64], F32, "sinb") for _ in range(3)]
    rAs = [kb.sb([128, 512], F32, "rA") for _ in range(2)]
    rBs = [kb.sb([128, 512], F32, "rB") for _ in range(2)]
    rCs = [kb.sb([128, 512], F32, "rC") for _ in range(2)]
    rDs = [kb.sb([128, 512], F32, "rD") for _ in range(2)]
    qrb = [kb.sb([128, D], BF16, "qrb") for _ in range(3)]
    krb = [kb.sb([128, 256], BF16, "krb") for _ in range(3)]
    vxb = [kb.sb([128, 2, 130], BF16, "vxb") for _ in range(3)]
    qTt = [kb.sb([128, 8, 128], BF16, "qTt") for _ in range(3)]
    kTt = [kb.sb([128, 2, 128], BF16, "kTt") for _ in range(3)]
    for bb in vxb:
        kb.op("pool", lambda e, bb=bb: e.memset(bb[:], 1.0), [], [bb])

    def sview(b_, nh, off):
        a_ = b_[:]
        return bass.AP(a_.tensor, a_.offset + off, [list(a_.ap[0]), [128, nh], [2, 64]])

    def cview(b_, nh):
        a_ = b_[:]
        return bass.AP(a_.tensor, a_.offset, [list(a_.ap[0]), [0, nh], [1, 64]])

    pcs = {"i": 0}

    def p1b_task(s, t0):
            is_ctx = t0 < TC
            v = NS if is_ctx else s
            k_ = pcs["i"] % 3
            pcs["i"] += 1
            sqb = sqbs[k_ % 2]
            rA, rB, rC, rD = rAs[k_ % 2], rBs[k_ % 2], rCs[k_ % 2], rDs[k_ % 2]
            xt = xt1[k_]
            hT = hT1[k_]
            ss = ssb[k_]
            kb.dma("sp", xt[:], x1_d[s][t0:t0 + 128, :], [x1_d[s]], [xt])
            norm_mod_T(xt, xt[:], 1, 0, v, hT, lambda j, hT=hT: hT[:, j, :])
            banks = []
            for b in ((2,) if is_ctx else (0, 1, 2)):
                pb = psum()
                banks.append((b, pb))
                for kc in range(KC):
                    kb.op("pe", lambda e, pb=pb, kc=kc, b=b, hT=hT: e.matmul(
                        pb[:, :], lhsT=hT[:, kc, :], rhs=wqb[:, kc, b * 512:(b + 1) * 512],
                        start=(kc == 0), stop=(kc == KC - 1)), [hT, wqb], [pb], inc=(kc == KC - 1))
            yield
            for (b, pb) in banks:
                if b < 2:
                    kb.op("act", lambda e, pb=pb: e.activation(out=sqb[:], in_=pb[:, :], func=AF.Square), [pb], [sqb])
                    kb.op("dve", lambda e, ss=ss, b=b: e.tensor_reduce(
                        out=ss[:, b * 4:(b + 1) * 4], in_=sqb[:].rearrange("p (h e) -> p h e", h=4), axis=AX.X, op=ALU.add),
                        [sqb], [ss])
                else:
                    kb.op("act", lambda e, pb=pb: e.activation(out=sqb[:, 0:256], in_=pb[:, 0:256], func=AF.Square), [pb], [sqb])
                    kb.op("dve", lambda e, ss=ss: e.tensor_reduce(
                        out=ss[:, 8:10], in_=sqb[:, 0:256].rearrange("p (h e) -> p h e", h=2), axis=AX.X, op=ALU.add),
                        [sqb], [ss])
            kb.op("dve", lambda e, ss=ss: e.tensor_scalar(out=ss[:, 0:10], in0=ss[:, 0:10], scalar1=1.0 / 128, scalar2=EPS,
                                                          op0=ALU.mult, op1=ALU.add), [ss], [ss])
            kb.op("act", lambda e, ss=ss: e.activation(out=ss[:, 0:10], in_=ss[:, 0:10], func=AF.Sqrt), [ss], [ss])
            kb.op("dve", lambda e, ss=ss: e.reciprocal(out=ss[:, 16:26], in_=ss[:, 0:10]), [ss], [ss])
            qn = qnb[k_]
            kn = knb[k_]
            vx = vxb[k_]
            for (b, pb) in banks:
                if b < 2:
                    kb.op("dve", lambda e, pb=pb, b=b, qn=qn, ss=ss: e.tensor_tensor(
                        out=qn[:, b * 512:(b + 1) * 512].rearrange("p (h e) -> p h e", h=4),
                        in0=pb[:, :].rearrange("p (h e) -> p h e", h=4),
                        in1=bass.AP(ss[:].tensor, ss[:].offset + 16 + b * 4, [list(ss[:].ap[0]), [1, 4], [0, 128]]),
                        op=ALU.mult), [pb, ss], [qn])
                else:
                    kb.op("dve", lambda e, pb=pb, kn=kn, ss=ss: e.tensor_tensor(
                        out=kn[:].rearrange("p (h e) -> p h e", h=2),
                        in0=pb[:, 0:256].rearrange("p (h e) -> p h e", h=2),
                        in1=bass.AP(ss[:].tensor, ss[:].offset + 24, [list(ss[:].ap[0]), [1, 2], [0, 128]]),
                        op=ALU.mult), [pb, ss], [kn])
                    kb.op("act", lambda e, pb=pb, vx=vx: e.copy(out=vx[:, :, 0:128],
                                                                in_=pb[:, 256:512].rearrange("p (h e) -> p h e", h=2)),
                          [pb], [vx])
            kb.op("pool", lambda e, kn=kn: e.tensor_tensor(out=kn[:].rearrange("p (h e) -> p h e", h=2),
                                                           in0=kn[:].rearrange("p (h e) -> p h e", h=2),
                                                           in1=bc_rows(krow, 2, 128), op=ALU.mult), [kn, krow], [kn])
            yield
            kr = krb[k_]
            if is_ctx:
                kb.op("act", lambda e, kr=kr, kn=kn: e.copy(out=kr[:], in_=kn[:]), [kn], [kr])
            else:
                qr = qrb[k_]
                cs_ = cosb[k_]
                sn_ = sinb[k_]
                tl0 = t0 - TC
                kb.dma("sp", cs_[:], cos_d[tl0:tl0 + 128, :], [cos_d], [cs_])
                kb.dma("sp", sn_[:], sin_d[tl0:tl0 + 128, :], [sin_d], [sn_])
                kb.op("pool", lambda e, qn=qn: e.tensor_tensor(out=qn[:].rearrange("p (h e) -> p h e", h=8),
                                                               in0=qn[:].rearrange("p (h e) -> p h e", h=8),
                                                               in1=bc_rows(qrow, 8, 128), op=ALU.mult), [qn, qrow], [qn])
                for (src_, dst_, nh) in ((qn, qr, 8), (kn, kr, 2)):
                    nn = nh * 64
                    vA = rA[:, 0:nn].rearrange("p (h i) -> p h i", h=nh)
                    vB = rB[:, 0:nn].rearrange("p (h i) -> p h i", h=nh)
                    vC = rC[:, 0:nn].rearrange("p (h i) -> p h i", h=nh)
                    vD = rD[:, 0:nn].rearrange("p (h i) -> p h i", h=nh)
                    x1v, x2v = sview(src_, nh, 0), sview(src_, nh, 1)
                    cv_, sv_ = cview(cs_, nh), cview(sn_, nh)
                    kb.op("dve", lambda e, vA=vA, x1v=x1v, cv_=cv_: e.tensor_tensor(out=vA, in0=x1v, in1=cv_, op=ALU.mult),
                          [src_, cs_], [rA])
                    kb.op("pool", lambda e, vB=vB, x2v=x2v, sv_=sv_: e.tensor_tensor(out=vB, in0=x2v, in1=sv_, op=ALU.mult),
                          [src_, sn_], [rB])
                    kb.op("pool", lambda e, vC=vC, x1v=x1v, sv_=sv_: e.tensor_tensor(out=vC, in0=x1v, in1=sv_, op=ALU.mult),
                          [src_, sn_], [rC])
                    kb.op("dve", lambda e, vD=vD, x2v=x2v, cv_=cv_: e.tensor_tensor(out=vD, in0=x2v, in1=cv_, op=ALU.mult),
                          [src_, cs_], [rD])
                    kb.op("dve", lambda e, dst_=dst_, nh=nh, vA=vA, vB=vB: e.tensor_tensor(
                        out=sview(dst_, nh, 0), in0=vA, in1=vB, op=ALU.subtract), [rA, rB], [dst_])
                    kb.op("pool", lambda e, dst_=dst_, nh=nh, vC=vC, vD=vD: e.tensor_tensor(
                        out=sview(dst_, nh, 1), in0=vC, in1=vD, op=ALU.add), [rC, rD], [dst_])
                yield
                pq = psum()
                pq16 = ps16(pq)
                for h in range(8):
                    kb.op("pe", lambda e, pq16=pq16, qr=qr, h=h: e.transpose(
                        out=pq16[:, h * 128:(h + 1) * 128], in_=qr[:, h * 128:(h + 1) * 128], identity=identb[:]),
                        [qr, identb], [pq], inc=(h == 7))
                qT_ = qTt[k_]
                kb.op("act", lambda e, qT_=qT_, pq16=pq16: e.copy(out=qT_[:].rearrange("p h t -> p (h t)"), in_=pq16[:, :]),
                      [pq], [qT_])
                kb.dma("pool", q1T_d[s][:, :, tl0:tl0 + 128], qT_[:], [qT_], [q1T_d[s]])
            pk = psum()
            pk16 = ps16(pk)
            for h in range(2):
                kb.op("pe", lambda e, pk16=pk16, kr=kr, h=h: e.transpose(
                    out=pk16[:, h * 128:(h + 1) * 128], in_=kr[:, h * 128:(h + 1) * 128], identity=identb[:]),
                    [kr, identb], [pk], inc=(h == 1))
            kT_ = kTt[k_]
            kb.op("dve", lambda e, kT_=kT_, pk16=pk16: e.tensor_copy(out=kT_[:].rearrange("p h t -> p (h t)"), in_=pk16[:, 0:256]),
                  [pk], [kT_])
            kb.dma("pool", k1T_d[s][:, :, t0:t0 + 128], kT_[:], [kT_], [k1T_d[s]])
            kb.dma("pool", v1_d[s][t0:t0 + 128, :, :], vx[:], [vx], [v1_d[s]])

    run_pipeline((p1b_task(s, t0) for s in range(NS) for t0 in range(0, T, 128)), 3)
    kb.pop()
    if STOP == "P1b":
        return finish()

    kb.push()
    K1 = kb.sb([128, 2, T], BF16, "K1")
    V1 = kb.sb([128, NT, 2, 130], BF16, "V1")
    Qb = [kb.sb([128, 8, 512], BF16, "Qb") for _ in range(2)]
    pTb = [kb.sb([128, 512], BF16, "pTb") for _ in range(3)]
    atok = [kb.sb([128, D], BF16, "atok") for _ in range(4)]
    rsb = [kb.sb([128, 4], F32, "rsb") for _ in range(2)]
    aTt = [kb.sb([128, 8, 128], BF16, "aTt") for _ in range(2)]
    mc = {"q": 0, "p": 0, "s": 0, "r": 0, "a": 0}
    for s in range(NS):
        kb.dma("sp", K1[:], k1T_d[s][:], [k1T_d[s]], [K1])
        kb.dma("sp", V1[:], v1_d[s][:].rearrange("(n p) h e -> p n h e", p=128), [v1_d[s]], [V1])
        for q0 in range(0, TL, 512):
            nq = min(512, TL - q0)
            nqt = nq // 128
            Q = Qb[mc["q"] % 2]
            mc["q"] += 1
            kb.dma("sp", Q[:, :, 0:nq], q1T_d[s][:, :, q0:q0 + nq], [q1T_d[s]], [Q])
            its = [(h, kc) for h in range(8) for kc in range(NT)]

            def qk(idx, Q=Q, nq=nq, its=its):
                h, kc = its[idx]
                kv = h // 4
                sT = PSB[4 + idx % 4]
                kb.op("pe", lambda e, sT=sT, kv=kv, kc=kc, h=h: e.matmul(
                    sT[:, 0:nq], lhsT=K1[:, kv, kc * 128:(kc + 1) * 128], rhs=Q[:, h, 0:nq], start=True, stop=True),
                    [K1, Q], [sT])

            LA = 2
            for idx in range(min(LA, len(its))):
                qk(idx)
            for idx, (h, kc) in enumerate(its):
                kv = h // 4
                if idx + LA < len(its):
                    qk(idx + LA)
                sT = PSB[4 + idx % 4]
                pT = pTb[idx % 3]
                kb.op("act", lambda e, pT=pT, sT=sT, nq=nq: e.activation(out=pT[:, 0:nq], in_=sT[:, 0:nq], func=AF.Exp),
                      [sT], [pT])
                for qt in range(nqt):
                    kb.op("pe", lambda e, qt=qt, pT=pT, kc=kc, kv=kv: e.matmul(
                        PSB[qt][:, 0:129], lhsT=pT[:, qt * 128:(qt + 1) * 128], rhs=V1[:, kc, kv, 0:129],
                        start=(kc == 0), stop=(kc == NT - 1)), [pT, V1], [PSB[qt]], inc=(qt == nqt - 1))
                if kc == NT - 1:
                    rs = rsb[mc["r"] % 2]
                    mc["r"] += 1
                    for qt in range(nqt):
                        kb.op("dve", lambda e, rs=rs, qt=qt: e.reciprocal(out=rs[:, qt:qt + 1], in_=PSB[qt][:, 128:129]),
                              [PSB[qt]], [rs])
                        kb.op("dve", lambda e, rs=rs, qt=qt, h=h: e.tensor_scalar(
                            out=atok[qt][:, h * 128:(h + 1) * 128], in0=PSB[qt][:, 0:128], scalar1=rs[:, qt:qt + 1],
                            scalar2=None, op0=ALU.mult), [PSB[qt], rs], [atok[qt]])
            for qt in range(nqt):
                pa = PSB[4 + mc["s"] % 4]
                mc["s"] += 1
                pa16 = ps16(pa)
                for h in range(8):
                    kb.op("pe", lambda e, pa16=pa16, qt=qt, h=h: e.transpose(
                        out=pa16[:, h * 128:(h + 1) * 128], in_=atok[qt][:, h * 128:(h + 1) * 128], identity=identb[:]),
                        [atok[qt], identb], [pa], inc=(h == 7))
                aT = aTt[mc["a"] % 2]
                mc["a"] += 1
                kb.op("dve", lambda e, aT=aT, pa16=pa16: e.tensor_copy(out=aT[:].rearrange("p h t -> p (h t)"), in_=pa16[:, :]),
                      [pa], [aT])
                tq = q0 + qt * 128
                kb.dma("pool", attT_d[s][:, tq:tq + 128].rearrange("(k p) t -> p k t", p=128), aT[:], [aT], [attT_d[s]])
    kb.pop()
    if STOP == "M1":
        return finish()

    post_phase(1)
    return finish()


def host_layout(inputs, core, NS, TC, TL):
    f = lambda a: np.ascontiguousarray(np.asarray(a, dtype=np.float32))
    b0 = core * NS
    m = {}
    m["x"] = f(inputs["x"][b0:b0 + NS, :TL])
    m["ctx"] = f(inputs["ctx"][b0:b0 + NS, :TC])
    cvs = np.concatenate([np.asarray(inputs["c"])[b0:b0 + NS], np.asarray(inputs["c_ctx"])[None]], 0)
    m["cvec"] = f(cvs.reshape(NS + 1, KC, 128).transpose(2, 1, 0))
    m["norm1c"] = f(np.asarray(inputs["norm1"]).reshape(2, KC, 128).transpose(0, 2, 1))
    m["norm2c"] = f(np.asarray(inputs["norm2"]).reshape(2, KC, 128).transpose(0, 2, 1))
    m["w_ada"] = f(np.asarray(inputs["w_ada"]).reshape(2, KC, 128, 6 * D).transpose(0, 2, 1, 3))
    m["b_adac"] = f(np.asarray(inputs["b_ada"]).reshape(2, 48, 128).transpose(0, 2, 1))
    m["w_in"] = f(np.asarray(inputs["ev_w_in"])[0].reshape(KC, 128, 2592).transpose(1, 0, 2))
    m["conv_w"] = f(np.asarray(inputs["ev_conv_w"])[0].reshape(4, 4, 128).transpose(2, 1, 0))
    m["conv_b"] = f(np.asarray(inputs["ev_conv_b"])[0].reshape(4, 128).T)
    for nm_, key in (("lru_wa", "ev_lru_wa"), ("lru_wi", "ev_lru_wi")):
        w = np.asarray(inputs[key])[0]
        bd = np.zeros((2, 4, 128, 128), np.float32)
        for d in range(2):
            for blk in range(8):
                cc, h = blk // 2, blk % 2
                bd[d, cc, h * 64:(h + 1) * 64, h * 64:(h + 1) * 64] = w[d, blk]
        m[nm_] = bd
    for nm_, key in (("lru_ba", "ev_lru_ba"), ("lru_bi", "ev_lru_bi"), ("lru_lam", "ev_lru_lam")):
        m[nm_] = f(np.asarray(inputs[key])[0].reshape(2, 4, 128).transpose(2, 0, 1))
    m["gla_wg"] = f(np.asarray(inputs["ev_gla_wg"])[0])
    m["gla_bg"] = f(np.asarray(inputs["ev_gla_bg"])[0].reshape(2, 2, 128).transpose(2, 0, 1))
    m["gla_norm"] = f(np.asarray(inputs["ev_gla_norm"])[0][None])
    m["w_out"] = f(np.asarray(inputs["ev_w_out"])[0].reshape(KC, 128, D).transpose(1, 0, 2))
    m["w_qkv"] = f(np.asarray(inputs["od_w_qkv"])[0].reshape(KC, 128, 1536).transpose(1, 0, 2))
    m["q_norm"] = f(np.asarray(inputs["od_q_norm"])[0][None])
    m["k_norm"] = f(np.asarray(inputs["od_k_norm"])[0][None])
    m["w_o"] = f(np.asarray(inputs["od_w_o"])[0].reshape(KC, 128, D).transpose(1, 0, 2))
    wr = np.concatenate([np.asarray(inputs["moe_wg"]), np.asarray(inputs["moe_we"])], -1)
    m["moe_wr"] = f(wr.reshape(2, KC, 128, 20).transpose(0, 2, 1, 3))
    m["moe_br"] = f(np.concatenate([np.asarray(inputs["moe_bg"]), np.asarray(inputs["moe_be"])], -1)[:, None, :])
    m["moe_w1"] = f(np.asarray(inputs["moe_w1"]).reshape(2, NEXP, KC, 128, HID).transpose(0, 1, 3, 2, 4))
    m["moe_w3"] = f(np.asarray(inputs["moe_w3"]).reshape(2, NEXP, KC, 128, HID).transpose(0, 1, 3, 2, 4))
    m["moe_w2"] = f(np.asarray(inputs["moe_w2"]).reshape(2, NEXP, 4, 128, D).transpose(0, 1, 3, 2, 4))
    m["ident"] = np.eye(128, dtype=np.float32)
    jj, ii = np.meshgrid(np.arange(128), np.arange(128), indexing="ij")
    m["maskf"] = (jj <= ii).astype(np.float32)
    m["maskb"] = (jj >= ii).astype(np.float32)
    t = np.arange(TL)
    row = (t // 64).astype(np.float32)
    col = (t % 64).astype(np.float32)
    freqs = (10000.0 ** (-np.arange(32, dtype=np.float32) / 32)).astype(np.float32)
    ang = np.concatenate([row[:, None] * freqs, col[:, None] * freqs], -1).astype(np.float32)
    m["rope_cos"] = np.cos(ang).astype(np.float32)
    m["rope_sin"] = np.sin(ang).astype(np.float32)
    return m


_CACHE = {}


def kernel(**inputs):
    NS, TC, TL = 2, 256, 4096
    ncores = 8
    cfg = {"NS": NS, "TC": TC, "TL": TL}
    nc = build(cfg)
    in_maps = [host_layout(inputs, c, NS, TC, TL) for c in range(ncores)]
    res = run_bass_kernel_spmd(nc, in_maps, core_ids=list(range(ncores)))
    out = np.concatenate([r["out"] for r in res.results], axis=0)
    return out.astype(np.float32)
```
